# Optimizing a Trainium2 kernel written in Bass

```python
import jax
import jax.numpy as jnp
from jax import lax
import numpy as np

D_MODEL = 2048
BATCH = 4
SEQ = 4096
DEPTH = 4

CTX_LEN = 256
GRID_W = 64
NORM_EPS = 1e-6
N_MOD = 6
F32 = jnp.float32

LRU_WIDTH = 512
LRU_BLOCKS = 4
LRU_BLOCK = LRU_WIDTH // LRU_BLOCKS
LRU_C = 8.0
CONV_W = 4

GDN_HEADS = 4
GDN_DK = 128
GDN_DV = 128
GDN_CHUNK = 64
GDN_QKV = GDN_HEADS * (2 * GDN_DK + GDN_DV)

FFT_GROUPS = 4
FFT_GROUP = 128
FFT_WIDTH = FFT_GROUPS * FFT_GROUP

MLA_HEADS = 4
MLA_Q_RANK = 512
MLA_KV_RANK = 256
MLA_NOPE = 128
MLA_ROPE = 64
MLA_V = 128
ROPE_BASE = 10000.0
Q_BLOCK = 128

N_BRANCH = 4
BRANCH_WIDTH = 512

N_EXPERTS = 32
TOP_K = 4
D_EXPERT = 512
SWIGLU_LIMIT = 7.0
SWIGLU_ALPHA = 1.702
MOE_BLOCK = 128

IN_SIZES = (
    LRU_WIDTH,
    LRU_WIDTH,
    GDN_QKV,
    GDN_HEADS * GDN_DV,
    2 * GDN_HEADS,
    2 * GDN_HEADS,
    FFT_WIDTH,
    MLA_Q_RANK,
    MLA_KV_RANK,
    MLA_ROPE,
    N_BRANCH * D_MODEL,
)
D_IN = sum(IN_SIZES)

kernel_name = "hybrid_diffusion_trunk_lru_gdn_fnet_mla_moe"


def rmsnorm(x, g):
    xf = x.astype(F32)
    y = xf * lax.rsqrt(jnp.mean(xf * xf, axis=-1, keepdims=True) + NORM_EPS)
    return (y * g.astype(F32)).astype(x.dtype)


def modulate(h, shift, scale):
    return h * (1.0 + scale) + shift


def l2norm(x):
    return x * lax.rsqrt(jnp.sum(x * x, axis=-1, keepdims=True) + NORM_EPS)


def split_cols(z):
    parts, start = [], 0
    for size in IN_SIZES:
        parts.append(z[..., start:start + size])
        start += size
    return parts


def centred_dwconv(x, w, b):
    k = w.shape[0]
    left = k // 2
    y = lax.conv_general_dilated(
        x, w[:, None, :].astype(x.dtype), window_strides=(1,),
        padding=[(left, k - 1 - left)], dimension_numbers=('NWC', 'WIO', 'NWC'),
        feature_group_count=x.shape[-1])
    return y + b.astype(x.dtype)


def linear_scan(a, b, h0, reverse):
    first, last = (-1, 0) if reverse else (0, -1)
    b = b.at[:, first].add(a[:, first] * h0)

    def combine(e1, e2):
        return e1[0] * e2[0], e2[0] * e1[1] + e2[1]

    _, h = lax.associative_scan(combine, (a, b), reverse=reverse, axis=1)
    return h, h[:, last]


def blockdiag(x, w):
    bn, t, _ = x.shape
    g, bi, bo = w.shape
    return jnp.einsum('btgi,gio->btgo', x.reshape(bn, t, g, bi), w).reshape(bn, t, g * bo)


def rglru_coeffs(x, w_a, b_a, w_x, b_x, lam):
    r = jax.nn.sigmoid(blockdiag(x, w_a) + b_a)
    i = jax.nn.sigmoid(blockdiag(x, w_x) + b_x)
    log_a = -LRU_C * r * jax.nn.softplus(-lam)
    return jnp.exp(log_a), jnp.sqrt(-jnp.expm1(2.0 * log_a)) * (i * x)


def rglru_mixer(u_ctx, u_lat, conv_w, conv_b, w_a, b_a, w_x, b_x, lam):
    x_ctx = centred_dwconv(u_ctx, conv_w, conv_b).astype(F32)
    x_lat = centred_dwconv(u_lat, conv_w, conv_b).astype(F32)
    w_a, b_a, w_x, b_x, lam = (t.astype(F32) for t in (w_a, b_a, w_x, b_x, lam))
    h0 = jnp.zeros((x_ctx.shape[0], LRU_WIDTH), F32)
    y_ctx = jnp.zeros_like(x_ctx)
    y_lat = jnp.zeros_like(x_lat)
    for d, rev in enumerate((False, True)):
        a_c, b_c = rglru_coeffs(x_ctx, w_a[d], b_a[d], w_x[d], b_x[d], lam[d])
        h_c, s_c = linear_scan(a_c, b_c, h0, rev)
        a_l, b_l = rglru_coeffs(x_lat, w_a[d], b_a[d], w_x[d], b_x[d], lam[d])
        h_l, _ = linear_scan(a_l, b_l, s_c, rev)
        y_ctx = y_ctx + h_c
        y_lat = y_lat + h_l
    return y_ctx, y_lat


def gdn_prepare(qkv, beta_logit, alpha_logit, conv_w, conv_b, a_log, dt_bias):
    bn, t, _ = qkv.shape
    x = jax.nn.silu(centred_dwconv(qkv, conv_w, conv_b)).astype(F32)
    nq = GDN_HEADS * GDN_DK
    q = l2norm(x[..., :nq].reshape(bn, t, GDN_HEADS, GDN_DK)) * GDN_DK ** -0.5
    k = l2norm(x[..., nq:2 * nq].reshape(bn, t, GDN_HEADS, GDN_DK))
    v = x[..., 2 * nq:].reshape(bn, t, GDN_HEADS, GDN_DV)
    beta = jax.nn.sigmoid(beta_logit.astype(F32)).reshape(bn, t, 2, GDN_HEADS)
    g = -jnp.exp(a_log.astype(F32)) * jax.nn.softplus(
        alpha_logit.astype(F32).reshape(bn, t, 2, GDN_HEADS) + dt_bias.astype(F32))
    to_bhtd = lambda z: jnp.transpose(z, (0, 2, 1, 3))
    to_dbht = lambda z: jnp.transpose(z, (2, 0, 3, 1))
    return to_bhtd(q), to_bhtd(k), to_bhtd(v), to_dbht(beta), to_dbht(g)


def gdn_chunked(q, k, v, beta, g, s0):
    bn, nh, t, dk = q.shape
    dv = v.shape[-1]
    nc, cs = t // GDN_CHUNK, GDN_CHUNK
    q = q.reshape(bn, nh, nc, cs, dk)
    k = k.reshape(bn, nh, nc, cs, dk)
    v = v.reshape(bn, nh, nc, cs, dv)
    beta = beta.reshape(bn, nh, nc, cs)
    gam = jnp.cumsum(g.reshape(bn, nh, nc, cs), axis=-1)
    diff = gam[..., :, None] - gam[..., None, :]
    idx = jnp.arange(cs)
    incl = idx[:, None] >= idx[None, :]
    strict = idx[:, None] > idx[None, :]
    dec_incl = jnp.exp(jnp.where(incl, diff, -jnp.inf))
    dec_strict = jnp.where(strict, dec_incl, 0.0)
    kb = k * beta[..., None]
    m = jnp.einsum('bhncd,bhnjd->bhncj', kb, k) * dec_strict + jnp.eye(cs, dtype=F32)
    rhs = jnp.concatenate([v * beta[..., None], kb * jnp.exp(gam)[..., None]], axis=-1)
    sol = lax.linalg.triangular_solve(m, rhs, left_side=True, lower=True, unit_diagonal=True)
    u, w = sol[..., :dv], sol[..., dv:]
    qk = jnp.einsum('bhncd,bhnjd->bhncj', q, k) * dec_incl
    q_dec = q * jnp.exp(gam)[..., None]
    k_dec = k * jnp.exp(gam[..., -1:] - gam)[..., None]
    g_tot = jnp.exp(gam[..., -1])
    xs = tuple(jnp.moveaxis(z, 2, 0) for z in (u, w, qk, q_dec, k_dec, g_tot))

    def step(s, inp):
        u_n, w_n, qk_n, qd_n, kd_n, gt_n = inp
        v_new = u_n - jnp.einsum('bhcd,bhde->bhce', w_n, s)
        o_n = jnp.einsum('bhcd,bhde->bhce', qd_n, s) + jnp.einsum('bhcj,bhje->bhce', qk_n, v_new)
        s = s * gt_n[..., None, None] + jnp.einsum('bhcd,bhce->bhde', kd_n, v_new)
        return s, o_n

    s_fin, o = lax.scan(step, s0, xs)
    return jnp.moveaxis(o, 0, 2).reshape(bn, nh, t, dv), s_fin


def gdn_mixer(qkv_ctx, beta_ctx, alpha_ctx, qkv_lat, beta_lat, alpha_lat, conv_w, conv_b, a_log, dt_bias):
    qc, kc, vc, bc, gc = gdn_prepare(qkv_ctx, beta_ctx, alpha_ctx, conv_w, conv_b, a_log, dt_bias)
    ql, kl, vl, bl, gl = gdn_prepare(qkv_lat, beta_lat, alpha_lat, conv_w, conv_b, a_log, dt_bias)
    s0 = jnp.zeros((qc.shape[0], GDN_HEADS, GDN_DK, GDN_DV), F32)
    o_ctx = jnp.zeros_like(vc)
    o_lat = jnp.zeros_like(vl)
    for d, rev in enumerate((False, True)):
        fl = (lambda z: jnp.flip(z, axis=2)) if rev else (lambda z: z)
        oc, sc = gdn_chunked(fl(qc), fl(kc), fl(vc), fl(bc[d]), fl(gc[d]), s0)
        ol, _ = gdn_chunked(fl(ql), fl(kl), fl(vl), fl(bl[d]), fl(gl[d]), sc)
        o_ctx = o_ctx + fl(oc)
        o_lat = o_lat + fl(ol)
    return o_ctx, o_lat


def gdn_output(o, z, g):
    bn, nh, t, dv = o.shape
    o = jnp.transpose(o, (0, 2, 1, 3))
    o = o * lax.rsqrt(jnp.mean(o * o, axis=-1, keepdims=True) + NORM_EPS) * g.astype(F32)
    o = o * jax.nn.silu(z.astype(F32)).reshape(bn, t, nh, dv)
    return o.reshape(bn, t, nh * dv).astype(z.dtype)


def fourier_mixer(u):
    bn, t, _ = u.shape
    uf = jnp.transpose(u.astype(F32).reshape(bn, t, FFT_GROUPS, FFT_GROUP), (0, 2, 1, 3))
    y = jnp.fft.fft2(uf, axes=(-2, -1), norm='ortho').real
    return jnp.transpose(y, (0, 2, 1, 3)).reshape(bn, t, FFT_WIDTH).astype(u.dtype)


def rope1d(x, pos):
    half = x.shape[-1] // 2
    inv = jnp.power(ROPE_BASE, -jnp.arange(half, dtype=F32) / half)
    ang = pos.astype(F32)[:, None] * inv
    cos = jnp.cos(ang)[:, None, :].astype(x.dtype)
    sin = jnp.sin(ang)[:, None, :].astype(x.dtype)
    x1, x2 = x[..., :half], x[..., half:]
    return jnp.concatenate([x1 * cos - x2 * sin, x2 * cos + x1 * sin], axis=-1)


def rope2d(x, row, col):
    half = x.shape[-1] // 2
    return jnp.concatenate([rope1d(x[..., :half], row), rope1d(x[..., half:], col)], axis=-1)


def mla_heads(cq, ckv, q_norm_g, kv_norm_g, w_uq, w_ukv):
    bn, t, _ = cq.shape
    q = (rmsnorm(cq, q_norm_g) @ w_uq).reshape(bn, t, MLA_HEADS, MLA_NOPE + MLA_ROPE)
    kv = (rmsnorm(ckv, kv_norm_g) @ w_ukv).reshape(bn, t, MLA_HEADS, MLA_NOPE + MLA_V)
    return q[..., :MLA_NOPE], q[..., MLA_NOPE:], kv[..., :MLA_NOPE], kv[..., MLA_NOPE:]


def mla_assemble(q_nope, q_rope, k_nope, k_rope, v):
    q = jnp.concatenate([q_nope, q_rope], axis=-1)
    k = jnp.concatenate([k_nope, jnp.broadcast_to(k_rope, k_nope.shape[:-1] + (MLA_ROPE,))], axis=-1)
    to_bhtd = lambda z: jnp.transpose(z, (0, 2, 1, 3))
    return to_bhtd(q), to_bhtd(k), to_bhtd(v)


def block_attention(q, k, v):
    bn, nh, tq, dq = q.shape
    nb = tq // Q_BLOCK
    qb = jnp.moveaxis(q.reshape(bn, nh, nb, Q_BLOCK, dq), 2, 0)
    scale = dq ** -0.5

    def one_block(qi):
        s = jnp.einsum('bhqd,bhkd->bhqk', qi, k).astype(F32) * scale
        p = jax.nn.softmax(s, axis=-1).astype(v.dtype)
        return jnp.einsum('bhqk,bhkd->bhqd', p, v)

    o = lax.map(one_block, qb)
    return jnp.moveaxis(o, 0, 2).reshape(bn, nh, tq, v.shape[-1])


def mla_mixer(cq_ctx, ckv_ctx, kr_ctx, cq_lat, ckv_lat, kr_lat, row, col, q_norm_g, kv_norm_g, w_uq, w_ukv):
    qn_c, qr_c, kn_c, v_c = mla_heads(cq_ctx, ckv_ctx, q_norm_g, kv_norm_g, w_uq, w_ukv)
    qn_l, qr_l, kn_l, v_l = mla_heads(cq_lat, ckv_lat, q_norm_g, kv_norm_g, w_uq, w_ukv)
    qr_l = rope2d(qr_l, row, col)
    kr_l = rope2d(kr_lat[:, :, None, :], row, col)
    q_c, k_c, v_c = mla_assemble(qn_c, qr_c, kn_c, kr_ctx[:, :, None, :], v_c)
    q_l, k_l, v_l = mla_assemble(qn_l, qr_l, kn_l, kr_l, v_l)
    o_c = block_attention(q_c, k_c, v_c)
    o_l = block_attention(q_l, jnp.concatenate([k_l, k_c], axis=2), jnp.concatenate([v_l, v_c], axis=2))
    to_btd = lambda o: jnp.transpose(o, (0, 2, 1, 3)).reshape(o.shape[0], o.shape[2], -1)
    return to_btd(o_c), to_btd(o_l)


def merge_branches(branches, gate_logits, w_branch, w_out):
    bn, t, _ = gate_logits.shape
    gates = jax.nn.sigmoid(gate_logits.astype(F32)).astype(gate_logits.dtype).reshape(bn, t, N_BRANCH, D_MODEL)
    merged = jnp.zeros((bn, t, D_MODEL), gate_logits.dtype)
    for i, y in enumerate(branches):
        merged = merged + gates[:, :, i] * (y @ w_branch[i])
    return merged @ w_out


def token_mixer(hn_ctx, hn_lat, row, col, w_in, lru_conv_w, lru_conv_b, lru_w_a, lru_b_a, lru_w_x, lru_b_x,
                lru_lambda, gdn_conv_w, gdn_conv_b, gdn_a_log, gdn_dt_bias, gdn_norm_g, mla_q_norm_g,
                mla_kv_norm_g, mla_w_uq, mla_w_ukv, w_branch, w_out):
    (lu_c, lg_c, qkv_c, z_c, be_c, al_c, fu_c, cq_c, ckv_c, kr_c, mg_c) = split_cols(hn_ctx @ w_in)
    (lu_l, lg_l, qkv_l, z_l, be_l, al_l, fu_l, cq_l, ckv_l, kr_l, mg_l) = split_cols(hn_lat @ w_in)
    ra_c, ra_l = rglru_mixer(lu_c, lu_l, lru_conv_w, lru_conv_b, lru_w_a, lru_b_a, lru_w_x, lru_b_x, lru_lambda)
    ya_c = jax.nn.gelu(lg_c) * ra_c.astype(lg_c.dtype)
    ya_l = jax.nn.gelu(lg_l) * ra_l.astype(lg_l.dtype)
    ob_c, ob_l = gdn_mixer(qkv_c, be_c, al_c, qkv_l, be_l, al_l, gdn_conv_w, gdn_conv_b, gdn_a_log, gdn_dt_bias)
    yb_c = gdn_output(ob_c, z_c, gdn_norm_g)
    yb_l = gdn_output(ob_l, z_l, gdn_norm_g)
    yc_c = fourier_mixer(fu_c)
    yc_l = fourier_mixer(fu_l)
    yd_c, yd_l = mla_mixer(cq_c, ckv_c, kr_c, cq_l, ckv_l, kr_l, row, col,
                           mla_q_norm_g, mla_kv_norm_g, mla_w_uq, mla_w_ukv)
    m_ctx = merge_branches((ya_c, yb_c, yc_c, yd_c), mg_c, w_branch, w_out)
    m_lat = merge_branches((ya_l, yb_l, yc_l, yd_l), mg_l, w_branch, w_out)
    return m_ctx, m_lat


def moe_ffn(xt, router_w, router_b, w1, b1, w2, b2):
    n, d = xt.shape
    logits = xt.astype(F32) @ router_w.astype(F32) + router_b.astype(F32)
    top_val, top_idx = lax.top_k(logits, TOP_K)
    gate = jax.nn.softmax(top_val, axis=-1)
    nk = n * TOP_K
    flat_e = top_idx.reshape(nk)
    flat_tok = jnp.repeat(jnp.arange(n, dtype=jnp.int32), TOP_K)
    order = jnp.argsort(flat_e)
    se, stok, sgate = flat_e[order], flat_tok[order], gate.reshape(nk)[order]
    counts = jnp.bincount(flat_e, length=N_EXPERTS)
    padded = (counts + MOE_BLOCK - 1) // MOE_BLOCK * MOE_BLOCK
    start = jnp.cumsum(counts) - counts
    pend = jnp.cumsum(padded)
    pstart = pend - padded
    dest = pstart[se] + jnp.arange(nk, dtype=jnp.int32) - start[se]
    n_blocks = (nk + N_EXPERTS * (MOE_BLOCK - 1) + MOE_BLOCK - 1) // MOE_BLOCK
    p = n_blocks * MOE_BLOCK
    buf_tok = jnp.zeros((p,), jnp.int32).at[dest].set(stok)
    buf_gate = jnp.zeros((p,), F32).at[dest].set(sgate)
    block_e = jnp.minimum(jnp.searchsorted(pend, jnp.arange(n_blocks, dtype=jnp.int32) * MOE_BLOCK,
                                           side='right'), N_EXPERTS - 1)

    def expert_block(args):
        tok, e = args
        h = xt[tok] @ w1[e] + b1[e]
        h_glu = jnp.minimum(h[:, :D_EXPERT], SWIGLU_LIMIT)
        h_lin = jnp.clip(h[:, D_EXPERT:], -SWIGLU_LIMIT, SWIGLU_LIMIT)
        act = h_glu * jax.nn.sigmoid(SWIGLU_ALPHA * h_glu) * (h_lin + 1.0)
        return act @ w2[e] + b2[e]

    out = lax.map(expert_block, (buf_tok.reshape(n_blocks, MOE_BLOCK), block_e))
    out = out.reshape(p, d) * buf_gate[:, None].astype(xt.dtype)
    return jnp.zeros_like(xt).at[buf_tok].add(out)


def setup_inputs(seed: int = 0) -> dict:
    key = jax.random.key(seed)
    keys = list(jax.random.split(key, 40))

    def nrm(shape, scale):
        return jax.random.normal(keys.pop(), shape, jnp.float32) * scale

    def unif(shape, lo, hi):
        return jax.random.uniform(keys.pop(), shape, jnp.float32, lo, hi)

    a_tgt = unif((DEPTH, 2, LRU_WIDTH), 0.9, 0.999)
    p_lam = jnp.power(a_tgt, 1.0 / LRU_C)
    dt = jnp.exp(unif((DEPTH, 2, GDN_HEADS), float(np.log(1e-3)), float(np.log(1e-1))))
    return {
        'x': nrm((BATCH, SEQ, D_MODEL), 1.0),
        'c': nrm((BATCH, D_MODEL), 1.0),
        'ctx': nrm((BATCH, CTX_LEN, D_MODEL), 1.0),
        'c_ctx': nrm((D_MODEL,), 1.0),
        'w_ada': nrm((DEPTH, D_MODEL, N_MOD * D_MODEL), 0.5 * D_MODEL ** -0.5),
        'b_ada': nrm((DEPTH, N_MOD * D_MODEL), 0.01),
        'norm1_g': 1.0 + nrm((DEPTH, D_MODEL), 0.01),
        'norm2_g': 1.0 + nrm((DEPTH, D_MODEL), 0.01),
        'w_in': nrm((DEPTH, D_MODEL, D_IN), D_MODEL ** -0.5),
        'lru_conv_w': nrm((DEPTH, CONV_W, LRU_WIDTH), CONV_W ** -0.5),
        'lru_conv_b': nrm((DEPTH, LRU_WIDTH), 0.01),
        'lru_w_a': nrm((DEPTH, 2, LRU_BLOCKS, LRU_BLOCK, LRU_BLOCK), LRU_BLOCK ** -0.5),
        'lru_b_a': nrm((DEPTH, 2, LRU_WIDTH), 0.01),
        'lru_w_x': nrm((DEPTH, 2, LRU_BLOCKS, LRU_BLOCK, LRU_BLOCK), LRU_BLOCK ** -0.5),
        'lru_b_x': nrm((DEPTH, 2, LRU_WIDTH), 0.01),
        'lru_lambda': jnp.log(p_lam) - jnp.log1p(-p_lam),
        'gdn_conv_w': nrm((DEPTH, CONV_W, GDN_QKV), CONV_W ** -0.5),
        'gdn_conv_b': nrm((DEPTH, GDN_QKV), 0.01),
        'gdn_a_log': jnp.log(unif((DEPTH, 2, GDN_HEADS), 1.0, 16.0)),
        'gdn_dt_bias': dt + jnp.log(-jnp.expm1(-dt)),
        'gdn_norm_g': 1.0 + nrm((DEPTH, GDN_DV), 0.01),
        'mla_q_norm_g': 1.0 + nrm((DEPTH, MLA_Q_RANK), 0.01),
        'mla_kv_norm_g': 1.0 + nrm((DEPTH, MLA_KV_RANK), 0.01),
        'mla_w_uq': nrm((DEPTH, MLA_Q_RANK, MLA_HEADS * (MLA_NOPE + MLA_ROPE)), MLA_Q_RANK ** -0.5),
        'mla_w_ukv': nrm((DEPTH, MLA_KV_RANK, MLA_HEADS * (MLA_NOPE + MLA_V)), MLA_KV_RANK ** -0.5),
        'w_branch': nrm((DEPTH, N_BRANCH, BRANCH_WIDTH, D_MODEL), BRANCH_WIDTH ** -0.5),
        'w_out': nrm((DEPTH, D_MODEL, D_MODEL), D_MODEL ** -0.5),
        'router_w': nrm((DEPTH, D_MODEL, N_EXPERTS), D_MODEL ** -0.5),
        'router_b': nrm((DEPTH, N_EXPERTS), 0.01),
        'exp_w1': nrm((DEPTH, N_EXPERTS, D_MODEL, 2 * D_EXPERT), D_MODEL ** -0.5),
        'exp_b1': nrm((DEPTH, N_EXPERTS, 2 * D_EXPERT), 0.01),
        'exp_w2': nrm((DEPTH, N_EXPERTS, D_EXPERT, D_MODEL), D_EXPERT ** -0.5),
        'exp_b2': nrm((DEPTH, N_EXPERTS, D_MODEL), 0.01),
        'final_norm_g': 1.0 + nrm((D_MODEL,), 0.01),
    }


def reference(x, c, ctx, c_ctx, w_ada, b_ada, norm1_g, norm2_g, w_in, lru_conv_w, lru_conv_b, lru_w_a, lru_b_a,
              lru_w_x, lru_b_x, lru_lambda, gdn_conv_w, gdn_conv_b, gdn_a_log, gdn_dt_bias, gdn_norm_g,
              mla_q_norm_g, mla_kv_norm_g, mla_w_uq, mla_w_ukv, w_branch, w_out, router_w, router_b,
              exp_w1, exp_b1, exp_w2, exp_b2, final_norm_g):
    n_lat = x.shape[1]
    rows = n_lat // GRID_W
    row = jnp.broadcast_to(jnp.arange(rows)[:, None], (rows, GRID_W)).reshape(n_lat)
    col = jnp.broadcast_to(jnp.arange(GRID_W)[None, :], (rows, GRID_W)).reshape(n_lat)
    h_lat, h_ctx = x, ctx
    s_lat, s_ctx = jax.nn.silu(c), jax.nn.silu(c_ctx)
    for l in range(DEPTH):
        last = l == DEPTH - 1
        mod_lat = jnp.split((s_lat @ w_ada[l] + b_ada[l])[:, None, :], N_MOD, axis=-1)
        mod_ctx = jnp.split((s_ctx @ w_ada[l] + b_ada[l])[None, None, :], N_MOD, axis=-1)
        hn_lat = modulate(rmsnorm(h_lat, norm1_g[l]), mod_lat[0], mod_lat[1])
        hn_ctx = modulate(rmsnorm(h_ctx, norm1_g[l]), mod_ctx[0], mod_ctx[1])
        m_ctx, m_lat = token_mixer(
            hn_ctx, hn_lat, row, col, w_in[l], lru_conv_w[l], lru_conv_b[l], lru_w_a[l], lru_b_a[l],
            lru_w_x[l], lru_b_x[l], lru_lambda[l], gdn_conv_w[l], gdn_conv_b[l], gdn_a_log[l],
            gdn_dt_bias[l], gdn_norm_g[l], mla_q_norm_g[l], mla_kv_norm_g[l], mla_w_uq[l], mla_w_ukv[l],
            w_branch[l], w_out[l])
        h_lat = h_lat + mod_lat[2] * m_lat
        hn_lat = modulate(rmsnorm(h_lat, norm2_g[l]), mod_lat[3], mod_lat[4])
        bl, tl, dm = h_lat.shape
        if last:
            f_lat = moe_ffn(hn_lat.reshape(bl * tl, dm), router_w[l], router_b[l],
                            exp_w1[l], exp_b1[l], exp_w2[l], exp_b2[l])
        else:
            h_ctx = h_ctx + mod_ctx[2] * m_ctx
            hn_ctx = modulate(rmsnorm(h_ctx, norm2_g[l]), mod_ctx[3], mod_ctx[4])
            n_ctx_tok = h_ctx.shape[0] * h_ctx.shape[1]
            tokens = jnp.concatenate([hn_ctx.reshape(n_ctx_tok, dm), hn_lat.reshape(bl * tl, dm)], axis=0)
            f_all = moe_ffn(tokens, router_w[l], router_b[l], exp_w1[l], exp_b1[l], exp_w2[l], exp_b2[l])
            h_ctx = h_ctx + mod_ctx[5] * f_all[:n_ctx_tok].reshape(h_ctx.shape)
            f_lat = f_all[n_ctx_tok:]
        h_lat = h_lat + mod_lat[5] * f_lat.reshape(h_lat.shape)
    return rmsnorm(h_lat, final_norm_g)
```

```python
import math
import numpy as np
import ml_dtypes
from contextlib import ExitStack
import concourse.bass as bass
import concourse.mybir as mybir
from concourse.bass_utils import run_bass_kernel_spmd


F32 = mybir.dt.float32
BF16 = mybir.dt.bfloat16
I32 = mybir.dt.int32
AF = mybir.ActivationFunctionType
ALU = mybir.AluOpType
AX = mybir.AxisListType

ENGS = ["pe", "act", "dve", "pool", "sp"]


class Prog:
    def __init__(self, nc, stack, n_dma_sems=24, same_engine_sync=True):
        self.nc = nc
        self.stack = stack
        self.ops = {e: [] for e in ENGS}
        self.sem = {e: stack.enter_context(nc.semaphore(f"s_{e}")) for e in ENGS}
        self.cnt = {e: 0 for e in ENGS}
        self.known = {e: {} for e in ENGS}
        self.last_w = {}
        self.readers = {}
        self.dma_sems = [stack.enter_context(nc.semaphore(f"s_dma{i}")) for i in range(n_dma_sems)]
        self.dma_val = [0] * n_dma_sems
        self.dma_rr = 0
        self.same_engine_sync = same_engine_sync
        self.out_toks = []
        self.n_sb = 0

    def sb(self, shape, dt=F32, name=None):
        self.n_sb += 1
        return self.stack.enter_context(self.nc.sbuf_tensor(f"{name or 'sb'}_u{self.n_sb}", list(shape), dt))

    def ps(self, shape, dt=F32, name=None):
        self.n_sb += 1
        return self.stack.enter_context(self.nc.psum_tensor(f"{name or 'ps'}_u{self.n_sb}", list(shape), dt))

    def _deps(self, eng, reads, writes):
        toks = []
        for k in reads:
            t = self.last_w.get(k)
            if t is not None:
                toks.append(t)
        for k in writes:
            t = self.last_w.get(k)
            if t is not None:
                toks.append(t)
            toks.extend(self.readers.get(k, []))
        waits = {}
        for (s, v, owner) in toks:
            if owner == eng and (not self.same_engine_sync or eng == "pe"):
                continue
            sid = id(s)
            if self.known[eng].get(sid, 0) >= v:
                continue
            if sid not in waits or waits[sid][1] < v:
                waits[sid] = (s, v)
        for sid, (s, v) in waits.items():
            self.known[eng][sid] = v
        return list(waits.values())

    def _commit(self, tok, reads, writes):
        for k in writes:
            self.last_w[k] = tok
            self.readers[k] = []
        for k in reads:
            if k in writes:
                continue
            self.readers.setdefault(k, []).append(tok)
            if len(self.readers[k]) > 12:
                best = {}
                for (s, v, o) in self.readers[k]:
                    if id(s) not in best or best[id(s)][1] < v:
                        best[id(s)] = (s, v, o)
                self.readers[k] = list(best.values())

    def op(self, eng, fn, reads=(), writes=()):
        waits = self._deps(eng, reads, writes)
        self.cnt[eng] += 1
        tok = (self.sem[eng], self.cnt[eng], eng)
        self.ops[eng].append((fn, waits, (self.sem[eng], 1)))
        self._commit(tok, reads, writes)
        return tok

    def dma(self, q, out, in_, reads=(), writes=(), is_output=False, **kw):
        i = self.dma_rr
        self.dma_rr = (self.dma_rr + 1) % len(self.dma_sems)
        s = self.dma_sems[i]
        waits = self._deps(q, reads, writes)
        if self.dma_val[i] > 0 and self.known[q].get(id(s), 0) < self.dma_val[i]:
            waits.append((s, self.dma_val[i]))
            self.known[q][id(s)] = self.dma_val[i]
        self.dma_val[i] += 16
        tok = (s, self.dma_val[i], "dma")
        self.ops[q].append((lambda e, o=out, a=in_, k=kw: e.dma_start(out=o, in_=a, **k), waits, (s, 16)))
        self._commit(tok, reads, writes)
        if is_output:
            self.out_toks.append(tok)
        return tok

    def finish(self):
        nc = self.nc
        final_waits = []
        seen = {}
        for (s, v, o) in self.out_toks:
            if id(s) not in seen or seen[id(s)][1] < v:
                seen[id(s)] = (s, v)
        final_waits = list(seen.values())
        ops = self.ops
        with nc.Block() as block:
            def emit(e, lst, extra=()):
                for (fn, waits, inc) in lst:
                    for (s, v) in waits:
                        e.wait_ge(s, v)
                    ins = fn(e)
                    ins.then_inc(inc[0], inc[1])
                for (s, v) in extra:
                    e.wait_ge(s, v)

            @block.tensor
            def _(e):
                emit(e, ops["pe"])

            @block.scalar
            def _(e):
                emit(e, ops["act"])

            @block.vector
            def _(e):
                emit(e, ops["dve"])

            @block.gpsimd
            def _(e):
                emit(e, ops["pool"])

            @block.sync
            def _(e):
                emit(e, ops["sp"], final_waits)


def _barrier(self):
    for e in ENGS:
        w = []
        for o in ENGS:
            if o != e and self.cnt[o] > self.known[e].get(id(self.sem[o]), 0):
                w.append((self.sem[o], self.cnt[o]))
                self.known[e][id(self.sem[o])] = self.cnt[o]
        for i, s in enumerate(self.dma_sems):
            if self.dma_val[i] > self.known[e].get(id(s), 0):
                w.append((s, self.dma_val[i]))
                self.known[e][id(s)] = self.dma_val[i]
        self.pending.setdefault(e, []).extend(w)
    self.last_w = {}
    self.readers = {}


Prog.barrier = _barrier
_old_init = Prog.__init__


def _init(self, *a, **k):
    _old_init(self, *a, **k)
    self.pending = {}


Prog.__init__ = _init
_old_op = Prog.op
_old_dma = Prog.dma


def _op(self, eng, fn, reads=(), writes=()):
    tok = _old_op(self, eng, fn, reads, writes)
    pw = self.pending.pop(eng, None)
    if pw:
        f, w, inc = self.ops[eng][-1]
        self.ops[eng][-1] = (f, pw + w, inc)
    return tok


def _dma(self, q, out, in_, reads=(), writes=(), is_output=False, **kw):
    tok = _old_dma(self, q, out, in_, reads, writes, is_output, **kw)
    pw = self.pending.pop(q, None)
    if pw:
        f, w, inc = self.ops[q][-1]
        self.ops[q][-1] = (f, pw + w, inc)
    return tok


Prog.op = _op
Prog.dma = _dma


def build_k0(nc):
    w = nc.dram_tensor("w", [2048, 6144], F32, kind="ExternalInput").ap()
    bias = nc.dram_tensor("bias", [128, 48], F32, kind="ExternalInput").ap()
    cT = nc.dram_tensor("cT", [128, 16, 8], F32, kind="ExternalInput").ap()
    modT = nc.dram_tensor("modT", [128, 48, 8], F32, kind="ExternalOutput").ap()
    with ExitStack() as st:
        P = Prog(nc, st)
        ct = P.sb([128, 16, 8], F32, "ct")
        sg = P.sb([128, 16, 8], F32, "sg")
        bs = P.sb([128, 48], F32, "bs")
        ob = P.sb([128, 48, 8], F32, "ob")
        wc = [P.sb([128, 16, 128], F32, f"wc{i}") for i in range(3)]
        pm = [P.ps([128, 512], F32, f"pm{i}") for i in range(2)]
        P.dma("sp", ct[:], cT, writes=["ct"])
        P.dma("sp", bs[:], bias, writes=["bs"])
        P.op("act", lambda e: e.activation(out=sg[:], in_=ct[:], func=AF.Sigmoid), reads=["ct"], writes=["sg"])
        P.op("dve", lambda e: e.tensor_tensor(out=sg[:], in0=sg[:], in1=ct[:], op=ALU.mult), reads=["sg", "ct"], writes=["sg"])
        wv = w.rearrange("(k p) c -> p k c", p=128)
        for j in range(48):
            wt = wc[j % 3]
            wk = f"wc{j % 3}"
            p_ = pm[j % 2]
            pk = f"pm{j % 2}"
            P.dma("sp" if j % 2 == 0 else "pool", wt[:], wv[:, :, j * 128:(j + 1) * 128], writes=[wk])
            for k in range(16):
                P.op("pe", lambda e, wt=wt, p_=p_, k=k: e.matmul(p_[:, 0:8], wt[:, k, :], sg[:, k, :], start=(k == 0), stop=(k == 15)), reads=[wk, "sg"], writes=[pk])
            P.op("dve", lambda e, p_=p_, j=j: e.tensor_scalar(out=ob[:, j, :], in0=p_[:, 0:8], scalar1=bs[:, j:j + 1], scalar2=None, op0=ALU.add), reads=[pk, "bs"], writes=["ob"])
        P.dma("sp", modT, ob[:], reads=["ob"], is_output=True)
        P.finish()
    return nc


D = 2048
NCH = 16
DIN = 12624
TL = 2176
BLOCKS = [(0, 128), (128, 512), (640, 512), (1152, 512), (1664, 512)]
EPS = 1e-6


def emit_norm_mod(P, st, h_src, vec, hn, ones_bf, pstag, htag, out_fp32=None):
    raise NotImplementedError


def build_k1(nc):
    hT = nc.dram_tensor("hT", [128, NCH, TL], F32, kind="ExternalInput").ap()
    vec = nc.dram_tensor("vec", [128, NCH, 5], F32, kind="ExternalInput").ap()
    w = nc.dram_tensor("w", [D, DIN], F32, kind="ExternalInput").ap()
    zT = nc.dram_tensor("zT", [DIN, TL], F32, kind="ExternalOutput").ap()
    with ExitStack() as st:
        P = Prog(nc, st)
        hn = P.sb([128, NCH, TL], BF16, "hn")
        vt = P.sb([128, NCH, 5], F32, "vt")
        al = P.sb([128, NCH, 4], F32, "al")
        ones = P.sb([128, 128], BF16, "ones")
        P.dma("sp", vt[:], vec, writes=["vt"])
        P.op("pool", lambda e: e.memset(ones[:], 1.0), writes=["ones"])
        P.op("dve", lambda e: e.scalar_tensor_tensor(out=al[:, :, 0], in0=vt[:, :, 1], scalar=1.0, in1=vt[:, :, 4], op0=ALU.add, op1=ALU.mult), reads=["vt"], writes=["al"])
        P.op("dve", lambda e: e.tensor_copy(out=al[:, :, 1], in_=vt[:, :, 0]), reads=["vt"], writes=["al"])
        P.op("dve", lambda e: e.scalar_tensor_tensor(out=al[:, :, 2], in0=vt[:, :, 3], scalar=1.0, in1=vt[:, :, 4], op0=ALU.add, op1=ALU.mult), reads=["vt"], writes=["al"])
        P.op("dve", lambda e: e.tensor_copy(out=al[:, :, 3], in_=vt[:, :, 2]), reads=["vt"], writes=["al"])
        emit_normmod(P, nc, lambda t0, n: hT[:, :, t0:t0 + n], al, ones, hn)
        emit_inproj(P, nc, hn, w, zT)
        P.finish()
    return nc


def emit_normmod(P, nc, h_src, al, ones, hn, tag="n1", blocks=None, out_dram=None):
    with ExitStack() as st2:
        old = P.stack
        P.stack = st2
        hb = [P.sb([128, NCH, 512], F32, f"{tag}_hb{i}") for i in range(2)]
        sq = P.sb([128, NCH, 512], BF16, f"{tag}_sq")
        rs = P.sb([128, 512], F32, f"{tag}_rs")
        rstd = P.sb([128, 512], F32, f"{tag}_rstd")
        tmp = [P.sb([128, 512], F32, f"{tag}_tmp{i}") for i in range(2)]
        pss = P.ps([128, 512], F32, f"{tag}_pss")
        for bi, (t0, n) in (blocks if blocks is not None else list(enumerate(BLOCKS))):
            h = hb[bi % 2]
            hk = f"{tag}_hb{bi % 2}"
            P.dma("sp", h[:, :, 0:n], h_src(t0, n), writes=[hk])
            P.op("act", lambda e, h=h, n=n: e.activation(out=sq[:, :, 0:n], in_=h[:, :, 0:n], func=AF.Square), reads=[hk], writes=[f"{tag}_sq"])
            for k in range(NCH):
                P.op("pe", lambda e, k=k, n=n: e.matmul(pss[:, 0:n], ones[:], sq[:, k, 0:n], start=(k == 0), stop=(k == NCH - 1)),
                     reads=[f"{tag}_sq", "ones"], writes=[f"{tag}_pss"])
            P.op("act", lambda e, n=n: e.activation(out=rs[:, 0:n], in_=pss[:, 0:n], func=AF.Sqrt, scale=1.0 / D, bias=EPS), reads=[f"{tag}_pss"], writes=[f"{tag}_rs"])
            P.op("dve", lambda e, n=n: e.reciprocal(out=rstd[:, 0:n], in_=rs[:, 0:n]), reads=[f"{tag}_rs"], writes=[f"{tag}_rstd"])
            ci = 2 if bi == 0 else 0
            hk = f"{tag}_hb{bi % 2}"
            for k in range(NCH):
                tb = tmp[k % 2]
                tk = f"{tag}_tmp{k % 2}"
                P.op("dve", lambda e, k=k, n=n, h=h, tb=tb: e.tensor_tensor(out=tb[:, 0:n], in0=h[:, k, 0:n], in1=rstd[:, 0:n], op=ALU.mult),
                     reads=[hk, f"{tag}_rstd"], writes=[tk])
                if out_dram is None:
                    P.op("act", lambda e, k=k, n=n, tb=tb, t0=t0, ci=ci: e.activation(out=hn[:, k, t0:t0 + n], in_=tb[:, 0:n], func=AF.Identity,
                                                                                   scale=al[:, k, ci:ci + 1], bias=al[:, k, ci + 1:ci + 2]),
                         reads=[tk, "al"], writes=[("hn", bi)])
                else:
                    P.op("act", lambda e, k=k, n=n, tb=tb, t0=t0, ci=ci: e.activation(out=tb[:, 0:n], in_=tb[:, 0:n], func=AF.Identity,
                                                                                   scale=al[:, k, ci:ci + 1], bias=al[:, k, ci + 1:ci + 2]),
                         reads=[tk, "al"], writes=[tk])
                    P.dma("sp", out_dram[:, k, t0:t0 + n], tb[:, 0:n], reads=[tk], is_output=True)
        P.barrier()
        P.stack = old


def emit_inproj(P, nc, hn, w, zT, tag="ip"):
    GW = 256
    ngroups = (DIN + GW - 1) // GW
    with ExitStack() as st2:
        old = P.stack
        P.stack = st2
        wst = [P.sb([128, NCH, GW], F32, f"{tag}_wst{i}") for i in range(2)]
        wbf = [P.sb([128, NCH, GW], BF16, f"{tag}_wbf{i}") for i in range(2)]
        zb = [P.sb([128, 512], F32, f"{tag}_zb{i}") for i in range(4)]
        pz = [P.ps([128, 512], F32, f"{tag}_pz{i}") for i in range(4)]
        wv = w.rearrange("(k p) c -> p k c", p=128)
        ctr = 0
        for g in range(ngroups):
            c0 = g * GW
            gw = min(GW, DIN - c0)
            ws, wb = wst[g % 2], wbf[g % 2]
            P.dma("sp" if g % 2 == 0 else "pool", ws[:, :, 0:gw], wv[:, :, c0:c0 + gw], writes=[f"{tag}_wst{g % 2}"])
            P.op("pool", lambda e, ws=ws, wb=wb, gw=gw: e.tensor_copy(out=wb[:, 0:8, 0:gw], in_=ws[:, 0:8, 0:gw]), reads=[f"{tag}_wst{g % 2}"], writes=[(f"{tag}_wbf{g % 2}", 0)])
            P.op("dve", lambda e, ws=ws, wb=wb, gw=gw: e.tensor_copy(out=wb[:, 8:16, 0:gw], in_=ws[:, 8:16, 0:gw]), reads=[f"{tag}_wst{g % 2}"], writes=[(f"{tag}_wbf{g % 2}", 1)])
            for s0 in range(0, gw, 128):
                cw = min(128, gw - s0)
                for bi, (t0, n) in enumerate(BLOCKS):
                    pi = ctr % 4
                    ctr += 1
                    for k in range(NCH):
                        P.op("pe", lambda e, k=k, wb=wb, s0=s0, cw=cw, t0=t0, n=n, pi=pi: e.matmul(pz[pi][0:cw, 0:n], wb[:, k, s0:s0 + cw], hn[:, k, t0:t0 + n], start=(k == 0), stop=(k == NCH - 1)),
                             reads=[(f"{tag}_wbf{g % 2}", 0), (f"{tag}_wbf{g % 2}", 1), ("hn", bi)], writes=[f"{tag}_pz{pi}"])
                    if ctr % 2 == 0:
                        P.op("act", lambda e, pi=pi, cw=cw, n=n: e.copy(out=zb[pi][0:cw, 0:n], in_=pz[pi][0:cw, 0:n]), reads=[f"{tag}_pz{pi}"], writes=[f"{tag}_zb{pi}"])
                    else:
                        P.op("dve", lambda e, pi=pi, cw=cw, n=n: e.tensor_copy(out=zb[pi][0:cw, 0:n], in_=pz[pi][0:cw, 0:n]), reads=[f"{tag}_pz{pi}"], writes=[f"{tag}_zb{pi}"])
                    P.dma("sp", zT[c0 + s0:c0 + s0 + cw, t0:t0 + n], zb[pi][0:cw, 0:n], reads=[f"{tag}_zb{pi}"], is_output=True)
        P.barrier()
        P.stack = old


T = 4352
TC = 256
TLAT = 4096
NT = 34
R_LU, R_LG, R_Q, R_K, R_V, R_ZG, R_BETA, R_ALPHA, R_FFT, R_CQ, R_CKV, R_KR, R_KRSW, R_END = 0, 256, 512, 768, 1024, 1280, 1536, 1544, 1552, 1808, 2320, 2576, 2640, 2704


class Sub:
    def __init__(self, P):
        self.P = P
    def __enter__(self):
        self.st = ExitStack()
        self.st.__enter__()
        self.old = self.P.stack
        self.P.stack = self.st
        return self
    def __exit__(self, *a):
        self.P.barrier()
        self.P.stack = self.old
        return self.st.__exit__(*a)


def emit_fft(P, zs, yT, dftc, dfts, chan_cs):
    with Sub(P):
        cs = P.sb([128, 256], F32, "f_cs")
        U = P.sb([128, T], F32, "f_U")
        PQ = [P.sb([128, NT, 256], BF16, f"f_PQ{g}") for g in range(2)]
        Cs = [P.sb([128, 2048], BF16, f"f_Cs{i}") for i in range(2)]
        Ss = [P.sb([128, 2048], BF16, f"f_Ss{i}") for i in range(2)]
        yb = [P.sb([128, 512], F32, f"f_yb{i}") for i in range(2)]
        acc = [P.ps([128, 512], F32, f"f_acc{i}") for i in range(8)]
        P.dma("sp", cs[:], chan_cs, writes=["f_cs"])
        n = 0
        for g in range(2):
            P.dma("sp", U[:], zs[R_FFT + g * 128:R_FFT + (g + 1) * 128, :], writes=["f_U"])
            for tt in range(NT):
                a = acc[tt % 2]
                ak = f"f_acc{tt % 2}"
                P.op("pe", lambda e, a=a, tt=tt: e.matmul(a[:, 0:256], U[:, tt * 128:(tt + 1) * 128], cs[:], start=True, stop=True), reads=["f_U", "f_cs"], writes=[ak])
                if tt % 2 == 0:
                    P.op("act", lambda e, a=a, tt=tt, g=g: e.copy(out=PQ[g][:, tt, :], in_=a[:, 0:256]), reads=[ak], writes=[(f"f_PQ{g}", tt)])
                else:
                    P.op("dve", lambda e, a=a, tt=tt, g=g: e.tensor_copy(out=PQ[g][:, tt, :], in_=a[:, 0:256]), reads=[ak], writes=[(f"f_PQ{g}", tt)])
        yi = 0
        for jh in range(2):
            for tl in range(32):
                tt = tl + 2
                i = tl % 2
                P.dma("sp", Cs[i][:], dftc[tl * 128:(tl + 1) * 128, jh * 2048:(jh + 1) * 2048], writes=[f"f_Cs{i}"])
                P.dma("pool", Ss[i][:], dfts[tl * 128:(tl + 1) * 128, jh * 2048:(jh + 1) * 2048], writes=[f"f_Ss{i}"])
                for g in range(2):
                    for j in range(4):
                        a = acc[g * 4 + j]
                        ak = f"f_acc{g * 4 + j}"
                        P.op("pe", lambda e, a=a, g=g, tt=tt, i=i, j=j, tl=tl: e.matmul(a[:], PQ[g][:, tt, 0:128], Cs[i][:, j * 512:(j + 1) * 512], start=(tl == 0), stop=False),
                             reads=[(f"f_PQ{g}", tt), f"f_Cs{i}"], writes=[ak])
                        P.op("pe", lambda e, a=a, g=g, tt=tt, i=i, j=j, tl=tl: e.matmul(a[:], PQ[g][:, tt, 128:256], Ss[i][:, j * 512:(j + 1) * 512], start=False, stop=(tl == 31)),
                             reads=[(f"f_PQ{g}", tt), f"f_Ss{i}"], writes=[ak])
            sc = 1.0 / math.sqrt(TLAT * 128)
            for g in range(2):
                for j in range(4):
                    a = acc[g * 4 + j]
                    ak = f"f_acc{g * 4 + j}"
                    y = yb[yi % 2]
                    yk = f"f_yb{yi % 2}"
                    yi += 1
                    P.op("act", lambda e, a=a, y=y, sc=sc: e.activation(out=y[:], in_=a[:], func=AF.Copy, scale=sc), reads=[ak], writes=[yk])
                    c0 = TC + jh * 2048 + j * 512
                    P.dma("sp", yT[2, g * 128:(g + 1) * 128, c0:c0 + 512], y[:], reads=[yk], is_output=True)
        for tt in range(2):
            i = tt % 2
            P.dma("sp", Cs[i][:, 0:256], dftc[tt * 2048:(tt + 1) * 2048:16, 0:256], writes=[f"f_Cs{i}"])
            P.dma("pool", Ss[i][:, 0:256], dfts[tt * 2048:(tt + 1) * 2048:16, 0:256], writes=[f"f_Ss{i}"])
            for g in range(2):
                a = acc[g]
                ak = f"f_acc{g}"
                P.op("pe", lambda e, a=a, g=g, tt=tt, i=i: e.matmul(a[:, 0:256], PQ[g][:, tt, 0:128], Cs[i][:, 0:256], start=(tt == 0), stop=False), reads=[(f"f_PQ{g}", tt), f"f_Cs{i}"], writes=[ak])
                P.op("pe", lambda e, a=a, g=g, tt=tt, i=i: e.matmul(a[:, 0:256], PQ[g][:, tt, 128:256], Ss[i][:, 0:256], start=False, stop=(tt == 1)), reads=[(f"f_PQ{g}", tt), f"f_Ss{i}"], writes=[ak])
        sc = 1.0 / math.sqrt(TC * 128)
        for g in range(2):
            y = yb[g]
            P.op("act", lambda e, g=g, y=y, sc=sc: e.activation(out=y[:, 0:256], in_=acc[g][:, 0:256], func=AF.Copy, scale=sc), reads=[f"f_acc{g}"], writes=[f"f_yb{g}"])
            P.dma("sp", yT[2, g * 128:(g + 1) * 128, 0:TC], y[:, 0:256], reads=[f"f_yb{g}"], is_output=True)


def emit_lru(P, zs, yT, lru_cw, lru_wg, lru_vec):
    GC = math.sqrt(2.0 / math.pi)
    with Sub(P):
        cw = P.sb([128, 2, 5], F32, "l_cw")
        wg = P.sb([128, 2, 2, 2, 128], F32, "l_wg")
        vv = P.sb([128, 2, 2, 3], F32, "l_vv")
        sc1 = P.sb([128, 2, 2, 2], F32, "l_sc")
        X, XC, Rr, Ii, TMP, HF, HB, LG = [P.sb([128, T], F32, f"l_{n}") for n in ["X", "XC", "R", "I", "TMP", "HF", "HB", "LG"]]
        pg = [P.ps([128, 512], F32, f"l_pg{i}") for i in range(4)]
        P.dma("sp", cw[:], lru_cw, writes=["l_cw"])
        P.dma("sp", wg[:], lru_wg, writes=["l_wg"])
        P.dma("sp", vv[:], lru_vec, writes=["l_vv"])
        P.op("act", lambda e: e.activation(out=sc1[:, :, :, 0], in_=vv[:, :, :, 2], func=AF.Exp, scale=-1.0), reads=["l_vv"], writes=["l_sc"])
        P.op("act", lambda e: e.activation(out=sc1[:, :, :, 0], in_=sc1[:, :, :, 0], func=AF.Ln, bias=1.0), reads=["l_sc"], writes=["l_sc"])
        P.op("dve", lambda e: e.tensor_scalar(out=sc1[:, :, :, 1], in0=sc1[:, :, :, 0], scalar1=-16.0, scalar2=None, op0=ALU.mult), reads=["l_sc"], writes=["l_sc"])
        P.op("dve", lambda e: e.tensor_scalar(out=sc1[:, :, :, 0], in0=sc1[:, :, :, 0], scalar1=-8.0, scalar2=None, op0=ALU.mult), reads=["l_sc"], writes=["l_sc"])
        segs = [(0, TC), (TC, T)]
        for g in range(2):
            P.dma("sp", X[:], zs[R_LU + g * 128:R_LU + (g + 1) * 128, :], writes=["l_X"])
            P.dma("pool", LG[:], zs[R_LG + g * 128:R_LG + (g + 1) * 128, :], writes=["l_LG"])
            P.op("act", lambda e, g=g: e.activation(out=XC[:], in_=X[:], func=AF.Identity, scale=cw[:, g, 2:3], bias=cw[:, g, 4:5]), reads=["l_X", "l_cw"], writes=["l_XC"])
            for (s0, s1) in segs:
                for j, off in ((0, -2), (1, -1), (3, 1)):
                    o0 = max(s0, s0 - off)
                    o1 = min(s1, s1 - off)
                    P.op("dve", lambda e, g=g, j=j, off=off, o0=o0, o1=o1: e.scalar_tensor_tensor(out=XC[:, o0:o1], in0=X[:, o0 + off:o1 + off], scalar=cw[:, g, j:j + 1], in1=XC[:, o0:o1], op0=ALU.mult, op1=ALU.add),
                         reads=["l_X", "l_XC", "l_cw"], writes=["l_XC"])
            for d in range(2):
                n = 0
                for b0 in range(0, T, 512):
                    nb = min(512, T - b0)
                    for gi, dst in ((0, Rr), (1, Ii)):
                        p = pg[n % 4]
                        pk = f"l_pg{n % 4}"
                        n += 1
                        P.op("pe", lambda e, p=p, g=g, d=d, gi=gi, b0=b0, nb=nb: e.matmul(p[:, 0:nb], wg[:, g, d, gi, :], XC[:, b0:b0 + nb], start=True, stop=True), reads=["l_wg", "l_XC"], writes=[pk])
                        dk = "l_R" if gi == 0 else "l_I"
                        P.op("act", lambda e, p=p, dst=dst, g=g, d=d, gi=gi, b0=b0, nb=nb: e.activation(out=dst[:, b0:b0 + nb], in_=p[:, 0:nb], func=AF.Sigmoid, bias=vv[:, g, d, gi:gi + 1]), reads=[pk, "l_vv"], writes=[dk])
                P.op("act", lambda e, g=g, d=d: e.activation(out=TMP[:], in_=Rr[:], func=AF.Exp, scale=sc1[:, g, d, 1:2]), reads=["l_R", "l_sc"], writes=["l_TMP"])
                P.op("act", lambda e, g=g, d=d: e.activation(out=Rr[:], in_=Rr[:], func=AF.Exp, scale=sc1[:, g, d, 0:1]), reads=["l_R", "l_sc"], writes=["l_R"])
                P.op("act", lambda e: e.activation(out=TMP[:], in_=TMP[:], func=AF.Sqrt, scale=-1.0, bias=1.0), reads=["l_TMP"], writes=["l_TMP"])
                P.op("pool", lambda e: e.tensor_tensor(out=Ii[:], in0=Ii[:], in1=XC[:], op=ALU.mult), reads=["l_I", "l_XC"], writes=["l_I"])
                P.op("dve", lambda e: e.tensor_tensor(out=Ii[:], in0=Ii[:], in1=TMP[:], op=ALU.mult), reads=["l_I", "l_TMP"], writes=["l_I"])
                if d == 0:
                    P.op("dve", lambda e: e.tensor_tensor_scan(out=HF[:], data0=Rr[:], data1=Ii[:], initial=0.0, op0=ALU.mult, op1=ALU.add), reads=["l_R", "l_I"], writes=["l_HF"])
                else:
                    P.op("dve", lambda e: e.tensor_tensor_scan(out=HB[:, TC - 1::-1], data0=Rr[:, TC - 1::-1], data1=Ii[:, TC - 1::-1], initial=0.0, op0=ALU.mult, op1=ALU.add), reads=["l_R", "l_I"], writes=["l_HB"])
                    P.op("dve", lambda e: e.tensor_tensor_scan(out=HB[:, T - 1:TC - 1:-1], data0=Rr[:, T - 1:TC - 1:-1], data1=Ii[:, T - 1:TC - 1:-1], initial=HB[:, 0:1], op0=ALU.mult, op1=ALU.add), reads=["l_R", "l_I", "l_HB"], writes=["l_HB"])
            P.op("pool", lambda e: e.tensor_tensor(out=HF[:], in0=HF[:], in1=HB[:], op=ALU.add), reads=["l_HF", "l_HB"], writes=["l_HF"])
            P.op("act", lambda e: e.activation(out=TMP[:], in_=LG[:], func=AF.Square), reads=["l_LG"], writes=["l_TMP"])
            P.op("dve", lambda e: e.tensor_scalar(out=TMP[:], in0=TMP[:], scalar1=0.044715, scalar2=1.0, op0=ALU.mult, op1=ALU.add), reads=["l_TMP"], writes=["l_TMP"])
            P.op("dve", lambda e: e.tensor_tensor(out=TMP[:], in0=TMP[:], in1=LG[:], op=ALU.mult), reads=["l_TMP", "l_LG"], writes=["l_TMP"])
            P.op("act", lambda e: e.activation(out=TMP[:], in_=TMP[:], func=AF.Sigmoid, scale=2.0 * GC), reads=["l_TMP"], writes=["l_TMP"])
            P.op("pool", lambda e: e.tensor_tensor(out=TMP[:], in0=TMP[:], in1=LG[:], op=ALU.mult), reads=["l_TMP", "l_LG"], writes=["l_TMP"])
            P.op("dve", lambda e: e.tensor_tensor(out=HF[:], in0=HF[:], in1=TMP[:], op=ALU.mult), reads=["l_TMP", "l_HF"], writes=["l_HF"])
            P.dma("sp", yT[0, g * 128:(g + 1) * 128, :], HF[:], reads=["l_HF"], is_output=True)


def emit_mla(P, zs, yT, mla_g, wuq, wuq_sw, wukv, rope_cos, rope_sin):
    SCALE = 192 ** -0.5
    QB = [(0, 256)] + [(TC + i * 512, 512) for i in range(8)]
    with Sub(P):
        gq = P.sb([128, 6], F32, "m_g")
        ones = P.sb([128, 128], BF16, "m_ones")
        COS = P.sb([64, TLAT], F32, "m_cos")
        SIN = P.sb([64, TLAT], F32, "m_sin")
        wq_st = P.sb([128, 4, 384], F32, "m_wqst")
        wq = P.sb([128, 4, 384], BF16, "m_wq")
        wqs_st = P.sb([128, 4, 128], F32, "m_wqsst")
        wqs = P.sb([128, 4, 128], BF16, "m_wqs")
        wkv_st = P.sb([128, 2, 512], F32, "m_wkvst")
        wkv = P.sb([128, 2, 512], BF16, "m_wkv")
        QN = [P.sb([128, T], BF16, f"m_QN{h}") for h in range(2)]
        QR = [P.sb([64, T], BF16, f"m_QR{h}") for h in range(2)]
        KN = [P.sb([128, T], BF16, f"m_KN{h}") for h in range(2)]
        V = [P.sb([128, NT, 128], BF16, f"m_V{h}") for h in range(2)]
        KR = P.sb([64, T], BF16, "m_KR")
        cqb = P.sb([128, 4, 512], F32, "m_cqb")
        ckvb = P.sb([128, 2, 512], F32, "m_ckvb")
        krb = P.sb([64, 2, 512], F32, "m_krb")
        sq = P.sb([128, 6, 512], BF16, "m_sq")
        rs = P.sb([128, 2, 512], F32, "m_rs")
        cqn = P.sb([128, 4, 512], BF16, "m_cqn")
        ckvn = P.sb([128, 2, 512], BF16, "m_ckvn")
        t1 = P.sb([64, 512], F32, "m_t1")
        t2 = P.sb([64, 512], F32, "m_t2")
        PT = [P.sb([128, 512], BF16, f"m_PT{i}") for i in range(3)]
        rl = P.sb([128, 512], F32, "m_rl")
        ob = [P.sb([128, 512], F32, f"m_ob{i}") for i in range(2)]
        ps = [P.ps([128, 512], F32, f"m_ps{i}") for i in range(4)]
        pS = [P.ps([128, 512], F32, f"m_pS{i}") for i in range(2)]
        pO = P.ps([128, 512], F32, "m_pO")
        pL = P.ps([128, 512], F32, "m_pL")
        P.dma("sp", gq[:], mla_g, writes=["m_g"])
        P.dma("sp", COS[:], rope_cos, writes=["m_cos"])
        P.dma("sp", SIN[:], rope_sin, writes=["m_sin"])
        P.dma("sp", wq_st[:], wuq.rearrange("(k p) c -> p k c", p=128), writes=["m_wqst"])
        P.dma("sp", wqs_st[:], wuq_sw.rearrange("(k p) c -> p k c", p=128), writes=["m_wqsst"])
        P.dma("sp", wkv_st[:], wukv.rearrange("(k p) c -> p k c", p=128), writes=["m_wkvst"])
        P.op("pool", lambda e: e.memset(ones[:], 1.0), writes=["m_ones"])
        P.op("dve", lambda e: e.tensor_copy(out=wq[:], in_=wq_st[:]), reads=["m_wqst"], writes=["m_wq"])
        P.op("dve", lambda e: e.tensor_copy(out=wqs[:], in_=wqs_st[:]), reads=["m_wqsst"], writes=["m_wqs"])
        P.op("dve", lambda e: e.tensor_copy(out=wkv[:], in_=wkv_st[:]), reads=["m_wkvst"], writes=["m_wkv"])
        pc = [0]

        def nps():
            i = pc[0] % 4
            pc[0] += 1
            return ps[i], f"m_ps{i}"
        for bi, (t0, n) in enumerate(QB):
            lat = bi > 0
            p0 = t0 - TC
            P.dma("sp", cqb[:, :, 0:n], zs[R_CQ:R_CQ + 512, t0:t0 + n].rearrange("(k p) t -> p k t", p=128), writes=["m_cqb"])
            P.dma("pool", ckvb[:, :, 0:n], zs[R_CKV:R_CKV + 256, t0:t0 + n].rearrange("(k p) t -> p k t", p=128), writes=["m_ckvb"])
            P.dma("pool", krb[:, :, 0:n], zs[R_KR:R_KR + 128, t0:t0 + n].rearrange("(k p) t -> p k t", p=64), writes=["m_krb"])
            P.op("act", lambda e, n=n: e.activation(out=sq[:, 0:4, 0:n], in_=cqb[:, :, 0:n], func=AF.Square), reads=["m_cqb"], writes=["m_sq"])
            P.op("act", lambda e, n=n: e.activation(out=sq[:, 4:6, 0:n], in_=ckvb[:, :, 0:n], func=AF.Square), reads=["m_ckvb"], writes=["m_sq"])
            for (k0, k1, ri, dd) in ((0, 4, 0, 512.0), (4, 6, 1, 256.0)):
                pp_, pk = nps()
                for k in range(k0, k1):
                    P.op("pe", lambda e, pp_=pp_, k=k, n=n, k0=k0, k1=k1: e.matmul(pp_[:, 0:n], ones[:], sq[:, k, 0:n], start=(k == k0), stop=(k == k1 - 1)), reads=["m_sq", "m_ones"], writes=[pk])
                P.op("act", lambda e, pp_=pp_, ri=ri, n=n, dd=dd: e.activation(out=rs[:, ri, 0:n], in_=pp_[:, 0:n], func=AF.Sqrt, scale=1.0 / dd, bias=1e-6), reads=[pk], writes=["m_rs"])
            P.op("dve", lambda e, n=n: e.reciprocal(out=rs[:, :, 0:n], in_=rs[:, :, 0:n]), reads=["m_rs"], writes=["m_rs"])
            for k in range(4):
                P.op("dve", lambda e, k=k, n=n: e.scalar_tensor_tensor(out=cqn[:, k, 0:n], in0=cqb[:, k, 0:n], scalar=gq[:, k:k + 1], in1=rs[:, 0, 0:n], op0=ALU.mult, op1=ALU.mult), reads=["m_cqb", "m_rs", "m_g"], writes=["m_cqn"])
            for k in range(2):
                P.op("dve", lambda e, k=k, n=n: e.scalar_tensor_tensor(out=ckvn[:, k, 0:n], in0=ckvb[:, k, 0:n], scalar=gq[:, 4 + k:5 + k], in1=rs[:, 1, 0:n], op0=ALU.mult, op1=ALU.mult), reads=["m_ckvb", "m_rs", "m_g"], writes=["m_ckvn"])
            if lat:
                P.op("dve", lambda e, n=n, p0=p0: e.tensor_tensor(out=t1[:, 0:n], in0=krb[:, 0, 0:n], in1=COS[:, p0:p0 + n], op=ALU.mult), reads=["m_krb", "m_cos"], writes=["m_t1"])
                P.op("pool", lambda e, n=n, p0=p0: e.tensor_tensor(out=t2[:, 0:n], in0=krb[:, 1, 0:n], in1=SIN[:, p0:p0 + n], op=ALU.mult), reads=["m_krb", "m_sin"], writes=["m_t2"])
                P.op("dve", lambda e, n=n, t0=t0: e.tensor_tensor(out=KR[:, t0:t0 + n], in0=t1[:, 0:n], in1=t2[:, 0:n], op=ALU.add), reads=["m_t1", "m_t2"], writes=["m_KR"])
            else:
                P.op("dve", lambda e, n=n, t0=t0: e.tensor_copy(out=KR[:, t0:t0 + n], in_=krb[:, 0, 0:n]), reads=["m_krb"], writes=["m_KR"])
            for h in range(2):
                pp_, pk = nps()
                for k in range(4):
                    P.op("pe", lambda e, pp_=pp_, k=k, n=n, h=h: e.matmul(pp_[:, 0:n], wq[:, k, h * 192:h * 192 + 128], cqn[:, k, 0:n], start=(k == 0), stop=(k == 3)), reads=["m_wq", "m_cqn"], writes=[pk])
                P.op("act", lambda e, pp_=pp_, n=n, h=h, t0=t0: e.copy(out=QN[h][:, t0:t0 + n], in_=pp_[:, 0:n]), reads=[pk], writes=[f"m_QN{h}"])
                pp_, pk = nps()
                for k in range(4):
                    P.op("pe", lambda e, pp_=pp_, k=k, n=n, h=h: e.matmul(pp_[0:64, 0:n], wq[:, k, h * 192 + 128:h * 192 + 192], cqn[:, k, 0:n], start=(k == 0), stop=(k == 3)), reads=["m_wq", "m_cqn"], writes=[pk])
                if lat:
                    pp2, pk2 = nps()
                    for k in range(4):
                        P.op("pe", lambda e, pp2=pp2, k=k, n=n, h=h: e.matmul(pp2[0:64, 0:n], wqs[:, k, h * 64:h * 64 + 64], cqn[:, k, 0:n], start=(k == 0), stop=(k == 3)), reads=["m_wqs", "m_cqn"], writes=[pk2])
                    P.op("dve", lambda e, pp_=pp_, n=n, p0=p0: e.tensor_tensor(out=t1[:, 0:n], in0=pp_[0:64, 0:n], in1=COS[:, p0:p0 + n], op=ALU.mult), reads=[pk, "m_cos"], writes=["m_t1"])
                    P.op("dve", lambda e, pp2=pp2, n=n, p0=p0: e.tensor_tensor(out=t2[:, 0:n], in0=pp2[0:64, 0:n], in1=SIN[:, p0:p0 + n], op=ALU.mult), reads=[pk2, "m_sin"], writes=["m_t2"])
                    P.op("pool", lambda e, n=n, t0=t0, h=h: e.tensor_tensor(out=QR[h][:, t0:t0 + n], in0=t1[:, 0:n], in1=t2[:, 0:n], op=ALU.add), reads=["m_t1", "m_t2"], writes=[f"m_QR{h}"])
                else:
                    P.op("act", lambda e, pp_=pp_, n=n, h=h, t0=t0: e.copy(out=QR[h][:, t0:t0 + n], in_=pp_[0:64, 0:n]), reads=[pk], writes=[f"m_QR{h}"])
                pp_, pk = nps()
                for k in range(2):
                    P.op("pe", lambda e, pp_=pp_, k=k, n=n, h=h: e.matmul(pp_[:, 0:n], wkv[:, k, h * 256:h * 256 + 128], ckvn[:, k, 0:n], start=(k == 0), stop=(k == 1)), reads=["m_wkv", "m_ckvn"], writes=[pk])
                P.op("dve", lambda e, pp_=pp_, n=n, h=h, t0=t0: e.tensor_copy(out=KN[h][:, t0:t0 + n], in_=pp_[:, 0:n]), reads=[pk], writes=[f"m_KN{h}"])
                for s in range(n // 128):
                    tt = (t0 + s * 128) // 128
                    pp_, pk = nps()
                    for k in range(2):
                        P.op("pe", lambda e, pp_=pp_, k=k, s=s, h=h: e.matmul(pp_[:, 0:128], ckvn[:, k, s * 128:(s + 1) * 128], wkv[:, k, h * 256 + 128:h * 256 + 256], start=(k == 0), stop=(k == 1)), reads=["m_wkv", "m_ckvn"], writes=[pk])
                    P.op("act", lambda e, pp_=pp_, h=h, tt=tt: e.copy(out=V[h][:, tt, :], in_=pp_[:, 0:128]), reads=[pk], writes=[f"m_V{h}"])
        it = 0
        for h in range(2):
            for bi, (t0, n) in enumerate(QB):
                nk = NT if bi > 0 else 2
                for kt in range(nk):
                    S = pS[it % 2]
                    Sk = f"m_pS{it % 2}"
                    pt = PT[it % 3]
                    ptk = f"m_PT{it % 3}"
                    it += 1
                    P.op("pe", lambda e, S=S, h=h, kt=kt, t0=t0, n=n: e.matmul(S[:, 0:n], KN[h][:, kt * 128:(kt + 1) * 128], QN[h][:, t0:t0 + n], start=True, stop=False), reads=[f"m_KN{h}", f"m_QN{h}"], writes=[Sk])
                    P.op("pe", lambda e, S=S, h=h, kt=kt, t0=t0, n=n: e.matmul(S[:, 0:n], KR[:, kt * 128:(kt + 1) * 128], QR[h][:, t0:t0 + n], start=False, stop=True), reads=["m_KR", f"m_QR{h}"], writes=[Sk])
                    P.op("act", lambda e, S=S, pt=pt, n=n: e.activation(out=pt[:, 0:n], in_=S[:, 0:n], func=AF.Exp, scale=SCALE), reads=[Sk], writes=[ptk])
                    P.op("pe", lambda e, pt=pt, h=h, kt=kt, n=n, nk=nk: e.matmul(pO[:, 0:n], V[h][:, kt, :], pt[:, 0:n], start=(kt == 0), stop=(kt == nk - 1)), reads=[ptk, f"m_V{h}"], writes=["m_pO"])
                    P.op("pe", lambda e, pt=pt, kt=kt, n=n, nk=nk: e.matmul(pL[:, 0:n], ones[:], pt[:, 0:n], start=(kt == 0), stop=(kt == nk - 1)), reads=[ptk, "m_ones"], writes=["m_pL"])
                o = ob[bi % 2]
                okk = f"m_ob{bi % 2}"
                P.op("dve", lambda e, n=n: e.reciprocal(out=rl[:, 0:n], in_=pL[:, 0:n]), reads=["m_pL"], writes=["m_rl"])
                P.op("dve", lambda e, o=o, n=n: e.tensor_tensor(out=o[:, 0:n], in0=pO[:, 0:n], in1=rl[:, 0:n], op=ALU.mult), reads=["m_pO", "m_rl"], writes=[okk])
                P.dma("sp", yT[3, h * 128:(h + 1) * 128, t0:t0 + n], o[:, 0:n], reads=[okk], is_output=True)


def rev_slice(c):
    if c < 2:
        start = TC - 1 - 128 * c
    else:
        start = TC + TLAT - 1 - 128 * (c - 2)
    stop = start - 128
    return slice(start, stop if stop >= 0 else None, -1)


def emit_gdn(P, zs, yT, gdn_cw, gdn_sc, gdn_gn, bg_tm, gconst, NCH_DBG=NT, NDIR=2, STOP=99):
    with Sub(P):
        gc = P.sb([128, 6, 128], F32, "g_const")
        ident, Jm, tri, strict, ones = gc[:, 0, :], gc[:, 1, :], gc[:, 2, :], gc[:, 3, :], gc[:, 5, :]
        cw = P.sb([128, 6, 5], F32, "g_cw")
        scv = P.sb([128, 4, 2], F32, "g_scv")
        gn = P.sb([128, 1], F32, "g_gn")
        bg = P.sb([128, NT, 8], F32, "g_bg")
        beta = P.sb([128, 4, NT], F32, "g_beta")
        gcol = P.sb([128, 4, NT], F32, "g_gcol")
        gam = P.sb([128, 4, NT], F32, "g_gam")
        eg = P.sb([128, 4, NT], F32, "g_eg")
        ekd = P.sb([128, 4, NT], F32, "g_ekd")
        gtot = P.sb([128, 4, NT], F32, "g_gtot")
        bw = P.sb([128, 4, NT], F32, "g_bw")
        tA = P.sb([128, 4, NT], F32, "g_tA")
        tB = P.sb([128, 4, NT], F32, "g_tB")
        X = P.sb([128, T], F32, "g_X")
        QT = P.sb([128, T], F32, "g_QT")
        KT = P.sb([128, T], F32, "g_KT")
        VT = P.sb([128, T], F32, "g_VT")
        Of = P.sb([128, NT, 128], F32, "g_Of")
        Ob = P.sb([128, NT, 128], F32, "g_Ob")
        nb = 3
        ch = {n: [P.sb([128, 128], F32, f"g_{n}{i}") for i in range(nb)] for n in
              ["qc", "kc", "vc", "kbw", "kdec", "vb", "G1", "Dm", "DmT", "L", "XT", "X2", "XT2", "RT", "u", "wT", "qkT", "vnew", "o1"]}
        S = [P.sb([128, 128], F32, f"g_S{i}") for i in range(2)]
        ss = P.sb([128, 2], F32, "g_ss")
        osum = P.sb([128, 128], F32, "g_osum")
        on = P.sb([128, 128], F32, "g_on")
        pp = [P.ps([128, 512], F32, f"g_pp{i}") for i in range(6)]
        pq = [P.ps([128, 512], F32, f"g_pq{i}") for i in range(2)]
        P.dma("sp", gc[:], gconst, writes=["g_const"])
        P.dma("sp", cw[:], gdn_cw, writes=["g_cw"])
        P.dma("sp", scv[:], gdn_sc, writes=["g_scv"])
        P.dma("sp", gn[:], gdn_gn, writes=["g_gn"])
        P.dma("sp", bg[:], bg_tm, writes=["g_bg"])
        pctr = [0]

        def npp():
            i = pctr[0] % 6
            pctr[0] += 1
            return pp[i], f"g_pp{i}"
        bgv_b = bg[:, :, 0:4].rearrange("p c k -> p k c")
        bgv_a = bg[:, :, 4:8].rearrange("p c k -> p k c")
        P.op("act", lambda e: e.activation(out=beta[:], in_=bgv_b, func=AF.Sigmoid), reads=["g_bg"], writes=["g_beta"])
        for k in range(4):
            P.op("dve", lambda e, k=k: e.tensor_scalar(out=tA[:, k, :], in0=bg[:, :, 4 + k], scalar1=scv[:, k, 1:2], scalar2=None, op0=ALU.add), reads=["g_bg", "g_scv"], writes=["g_tA"])
        P.op("act", lambda e: e.activation(out=tB[:], in_=tA[:], func=AF.Abs), reads=["g_tA"], writes=["g_tB"])
        P.op("act", lambda e: e.activation(out=tB[:], in_=tB[:], func=AF.Exp, scale=-1.0), reads=["g_tB"], writes=["g_tB"])
        P.op("act", lambda e: e.activation(out=tB[:], in_=tB[:], func=AF.Ln, bias=1.0), reads=["g_tB"], writes=["g_tB"])
        P.op("dve", lambda e: e.scalar_tensor_tensor(out=tA[:], in0=tA[:], scalar=0.0, in1=tB[:], op0=ALU.max, op1=ALU.add), reads=["g_tA", "g_tB"], writes=["g_tA"])
        P.op("act", lambda e: e.activation(out=scv[:, :, 0], in_=scv[:, :, 0], func=AF.Exp), reads=["g_scv"], writes=["g_scv"])
        for k in range(4):
            P.op("dve", lambda e, k=k: e.tensor_scalar(out=gcol[:, k, :], in0=tA[:, k, :], scalar1=scv[:, k, 0:1], scalar2=-1.0, op0=ALU.mult, op1=ALU.mult), reads=["g_tA", "g_scv"], writes=["g_gcol"])
        p1, k1 = npp()
        P.op("pe", lambda e: e.matmul(p1[:, 0:4 * NT], tri, gcol[:].rearrange("p k c -> p (k c)"), start=True, stop=True), reads=["g_gcol", "g_const"], writes=[k1])
        P.op("dve", lambda e: e.tensor_copy(out=gam[:].rearrange("p k c -> p (k c)"), in_=p1[:, 0:4 * NT]), reads=[k1], writes=["g_gam"])
        p2, k2 = npp()
        P.op("pe", lambda e: e.matmul(p2[:, 0:4 * NT], ones, gcol[:].rearrange("p k c -> p (k c)"), start=True, stop=True), reads=["g_gcol", "g_const"], writes=[k2])
        P.op("act", lambda e: e.activation(out=gtot[:].rearrange("p k c -> p (k c)"), in_=p2[:, 0:4 * NT], func=AF.Exp), reads=[k2], writes=["g_gtot"])
        P.op("dve", lambda e: e.tensor_tensor(out=tB[:].rearrange("p k c -> p (k c)"), in0=p2[:, 0:4 * NT], in1=gam[:].rearrange("p k c -> p (k c)"), op=ALU.subtract), reads=[k2, "g_gam"], writes=["g_tB", k2])
        P.op("act", lambda e: e.activation(out=ekd[:], in_=tB[:], func=AF.Exp), reads=["g_tB"], writes=["g_ekd"])
        P.op("act", lambda e: e.activation(out=eg[:], in_=gam[:], func=AF.Exp), reads=["g_gam"], writes=["g_eg"])
        P.op("dve", lambda e: e.tensor_tensor(out=bw[:], in0=beta[:], in1=eg[:], op=ALU.mult), reads=["g_beta", "g_eg"], writes=["g_bw"])
        segs = [(0, TC), (TC, T)]
        cctr = 0
        for hl in range(2):
            for ti, (dst, dk_) in enumerate(((QT, "g_QT"), (KT, "g_KT"), (VT, "g_VT"))):
                ci = ti * 2 + hl
                r0 = (R_Q, R_K, R_V)[ti] + hl * 128
                P.dma("sp", X[:], zs[r0:r0 + 128, :], writes=["g_X"])
                P.op("act", lambda e, ci=ci, dst=dst: e.activation(out=dst[:], in_=X[:], func=AF.Identity, scale=cw[:, ci, 2:3], bias=cw[:, ci, 4:5]), reads=["g_X", "g_cw"], writes=[dk_])
                for (s0, s1) in segs:
                    for j, off in ((0, -2), (1, -1), (3, 1)):
                        o0 = max(s0, s0 - off)
                        o1 = min(s1, s1 - off)
                        P.op("dve", lambda e, ci=ci, j=j, off=off, o0=o0, o1=o1, dst=dst: e.scalar_tensor_tensor(out=dst[:, o0:o1], in0=X[:, o0 + off:o1 + off], scalar=cw[:, ci, j:j + 1], in1=dst[:, o0:o1], op0=ALU.mult, op1=ALU.add),
                             reads=["g_X", dk_, "g_cw"], writes=[dk_])
                P.op("act", lambda e, dst=dst: e.activation(out=X[:], in_=dst[:], func=AF.Sigmoid), reads=[dk_], writes=["g_X"])
                P.op("pool", lambda e, dst=dst: e.tensor_tensor(out=dst[:], in0=dst[:], in1=X[:], op=ALU.mult), reads=[dk_, "g_X"], writes=[dk_])
                if ti < 2:
                    P.op("act", lambda e, dst=dst: e.activation(out=X[:], in_=dst[:], func=AF.Square), reads=[dk_], writes=["g_X"])
                    for b0 in range(0, T, 512):
                        n = min(512, T - b0)
                        p_, pk = npp()
                        P.op("pe", lambda e, p_=p_, b0=b0, n=n: e.matmul(p_[:, 0:n], ones, X[:, b0:b0 + n], start=True, stop=True), reads=["g_X", "g_const"], writes=[pk])
                        P.op("act", lambda e, p_=p_, b0=b0, n=n, scl=(128.0 if ti == 0 else 1.0): e.activation(out=X[:, b0:b0 + n], in_=p_[:, 0:n], func=AF.Sqrt, bias=1e-6, scale=scl), reads=[pk, "g_X"], writes=["g_X"])
                    P.op("dve", lambda e: e.reciprocal(out=X[:], in_=X[:]), reads=["g_X"], writes=["g_X"])
                    P.op("pool", lambda e, dst=dst: e.tensor_tensor(out=dst[:], in0=dst[:], in1=X[:], op=ALU.mult), reads=[dk_, "g_X"], writes=[dk_])
            for d in range(NDIR):
                kd = d * 2 + hl
                Od, Odk = (Of, "g_Of") if d == 0 else (Ob, "g_Ob")
                P.op("dve", lambda e: e.memset(S[0][:], 0.0), writes=["g_S0"])
                for c in range(NCH_DBG):
                    bi = cctr % nb
                    cctr += 1
                    B = {n: ch[n][bi] for n in ch}
                    K_ = {n: f"g_{n}{bi}" for n in ch}
                    if d == 0:
                        sl = slice(c * 128, (c + 1) * 128)
                        qc, kc, vc = QT[:, sl], KT[:, sl], VT[:, sl]
                        rq, rk, rv = ["g_QT"], ["g_KT"], ["g_VT"]
                    else:
                        sl = rev_slice(c)
                        P.op("act", lambda e, B=B, sl=sl: e.copy(out=B["qc"][:], in_=QT[:, sl]), reads=["g_QT"], writes=[K_["qc"]])
                        P.op("dve", lambda e, B=B, sl=sl: e.tensor_copy(out=B["kc"][:], in_=KT[:, sl]), reads=["g_KT"], writes=[K_["kc"]])
                        P.op("act", lambda e, B=B, sl=sl: e.copy(out=B["vc"][:], in_=VT[:, sl]), reads=["g_VT"], writes=[K_["vc"]])
                        qc, kc, vc = B["qc"][:], B["kc"][:], B["vc"][:]
                        rq, rk, rv = [K_["qc"]], [K_["kc"]], [K_["vc"]]
                    cB = {nm: tbl[:, kd, c:c + 1] for nm, tbl in (("bw", bw), ("ekd", ekd), ("beta", beta), ("gcol", gcol), ("gam", gam), ("eg", eg), ("gtot", gtot))}
                    p_, pk = npp()
                    P.op("pe", lambda e, p_=p_, kc=kc: e.transpose(p_[:, 0:128], kc, ident), reads=rk + ["g_const"], writes=[pk])
                    P.op("act", lambda e, cB=cB, p_=p_, B=B: e.activation(out=B["kbw"][:], in_=p_[:, 0:128], func=AF.Identity, scale=cB["bw"]), reads=[pk, "g_bw"], writes=[K_["kbw"]])
                    P.op("dve", lambda e, cB=cB, p_=p_, B=B: e.tensor_scalar(out=B["kdec"][:], in0=p_[:, 0:128], scalar1=cB["ekd"], scalar2=None, op0=ALU.mult), reads=[pk, "g_ekd"], writes=[K_["kdec"], pk])
                    p_, pk = npp()
                    P.op("pe", lambda e, p_=p_, vc=vc: e.transpose(p_[:, 0:128], vc, ident), reads=rv + ["g_const"], writes=[pk])
                    P.op("act", lambda e, cB=cB, p_=p_, B=B: e.activation(out=B["vb"][:], in_=p_[:, 0:128], func=AF.Identity, scale=cB["beta"]), reads=[pk, "g_beta"], writes=[K_["vb"]])
                    if STOP <= 1:
                        continue
                    P.op("dve", lambda e, cB=cB, B=B: e.tensor_scalar(out=B["G1"][:], in0=tri, scalar1=cB["gcol"], scalar2=None, op0=ALU.mult), reads=["g_const", "g_gcol"], writes=[K_["G1"]])
                    pg_, pgk = npp()
                    P.op("pe", lambda e, pg_=pg_, B=B: e.matmul(pg_[:, 0:128], ones, B["G1"][:], start=True, stop=True), reads=[K_["G1"], "g_const"], writes=[pgk])
                    P.op("dve", lambda e, cB=cB, pg_=pg_, B=B: e.tensor_scalar(out=B["Dm"][:], in0=pg_[:, 0:128], scalar1=cB["gam"], scalar2=0.0, op0=ALU.subtract, op1=ALU.max), reads=[pgk, "g_gam"], writes=[K_["Dm"]])
                    P.op("act", lambda e, B=B: e.activation(out=B["Dm"][:], in_=B["Dm"][:], func=AF.Exp, scale=-1.0), reads=[K_["Dm"]], writes=[K_["Dm"]])
                    P.op("pool", lambda e, B=B: e.tensor_tensor(out=B["Dm"][:], in0=B["Dm"][:], in1=strict, op=ALU.mult), reads=[K_["Dm"], "g_const"], writes=[K_["Dm"]])
                    P.op("dve", lambda e, cB=cB, pg_=pg_, B=B: e.tensor_scalar(out=B["DmT"][:], in0=pg_[:, 0:128], scalar1=cB["gam"], scalar2=0.0, op0=ALU.subtract, op1=ALU.min), reads=[pgk, "g_gam"], writes=[K_["DmT"]])
                    P.op("act", lambda e, B=B: e.activation(out=B["DmT"][:], in_=B["DmT"][:], func=AF.Exp), reads=[K_["DmT"]], writes=[K_["DmT"]])
                    P.op("pool", lambda e, B=B: e.tensor_tensor(out=B["DmT"][:], in0=B["DmT"][:], in1=tri, op=ALU.mult), reads=[K_["DmT"], "g_const"], writes=[K_["DmT"]])
                    if STOP <= 2:
                        continue
                    p_, pk = npp()
                    P.op("pe", lambda e, p_=p_, kc=kc: e.matmul(p_[:, 0:128], kc, kc, start=True, stop=True), reads=rk, writes=[pk])
                    P.op("dve", lambda e, cB=cB, p_=p_, B=B: e.scalar_tensor_tensor(out=B["L"][:], in0=p_[:, 0:128], scalar=cB["beta"], in1=B["Dm"][:], op0=ALU.mult, op1=ALU.mult), reads=[pk, "g_beta", K_["Dm"]], writes=[K_["L"]])
                    p_, pk = npp()
                    P.op("pe", lambda e, p_=p_, kc=kc, qc=qc: e.matmul(p_[:, 0:128], kc, qc, start=True, stop=True), reads=rk + rq, writes=[pk])
                    P.op("dve", lambda e, p_=p_, B=B: e.tensor_tensor(out=B["qkT"][:], in0=p_[:, 0:128], in1=B["DmT"][:], op=ALU.mult), reads=[pk, K_["DmT"]], writes=[K_["qkT"]])
                    if STOP <= 3:
                        continue
                    p_, pk = npp()
                    P.op("pe", lambda e, p_=p_, B=B: e.transpose(p_[:, 0:128], B["L"][:], ident), reads=[K_["L"], "g_const"], writes=[pk])
                    P.op("act", lambda e, p_=p_, B=B: e.copy(out=B["XT"][:], in_=p_[:, 0:128]), reads=[pk], writes=[K_["XT"]])
                    P.op("dve", lambda e, p_=p_, B=B: e.scalar_tensor_tensor(out=B["RT"][:], in0=p_[:, 0:128], scalar=-1.0, in1=ident, op0=ALU.mult, op1=ALU.add), reads=[pk, "g_const"], writes=[K_["RT"], pk])
                    if STOP <= 4:
                        continue
                    Xc, Xk = B["L"], K_["L"]
                    XTc, XTk = B["XT"], K_["XT"]
                    Xn, Xnk = B["X2"], K_["X2"]
                    XTn, XTnk = B["XT2"], K_["XT2"]
                    for lev in range(6):
                        p_, pk = npp()
                        P.op("pe", lambda e, p_=p_, XTc=XTc, Xc=Xc: e.matmul(p_[:, 0:128], XTc[:], Xc[:], start=True, stop=True), reads=[XTk, Xk], writes=[pk])
                        P.op("act", lambda e, p_=p_, Xn=Xn: e.copy(out=Xn[:], in_=p_[:, 0:128]), reads=[pk], writes=[Xnk])
                        if lev < 5:
                            p3, pk3 = npp()
                            P.op("pe", lambda e, p3=p3, XTc=XTc, Xc=Xc: e.matmul(p3[:, 0:128], Xc[:], XTc[:], start=True, stop=True), reads=[XTk, Xk], writes=[pk3])
                            P.op("act", lambda e, p3=p3, XTn=XTn: e.copy(out=XTn[:], in_=p3[:, 0:128]), reads=[pk3], writes=[XTnk])
                        p4, pk4 = npp()
                        P.op("pe", lambda e, p4=p4, Xn=Xn, B=B: e.matmul(p4[:, 0:128], Xn[:], B["RT"][:], start=True, stop=True), reads=[Xnk, K_["RT"]], writes=[pk4])
                        P.op("dve", lambda e, p4=p4, B=B: e.tensor_tensor(out=B["RT"][:], in0=B["RT"][:], in1=p4[:, 0:128], op=ALU.add), reads=[pk4, K_["RT"]], writes=[K_["RT"]])
                        Xc, Xk, Xn, Xnk = Xn, Xnk, Xc, Xk
                        XTc, XTk, XTn, XTnk = XTn, XTnk, XTc, XTk
                    if STOP <= 5:
                        continue
                    p_, pk = npp()
                    P.op("pe", lambda e, p_=p_, B=B: e.matmul(p_[:, 0:128], B["RT"][:], B["vb"][:], start=True, stop=True), reads=[K_["RT"], K_["vb"]], writes=[pk])
                    P.op("act", lambda e, p_=p_, B=B: e.copy(out=B["u"][:], in_=p_[:, 0:128]), reads=[pk], writes=[K_["u"]])
                    p_, pk = npp()
                    P.op("pe", lambda e, p_=p_, B=B: e.matmul(p_[:, 0:128], B["kbw"][:], B["RT"][:], start=True, stop=True), reads=[K_["RT"], K_["kbw"]], writes=[pk])
                    P.op("act", lambda e, p_=p_, B=B: e.copy(out=B["wT"][:], in_=p_[:, 0:128]), reads=[pk], writes=[K_["wT"]])
                    if STOP <= 6:
                        continue
                    Sc, Sk = S[c % 2], f"g_S{c % 2}"
                    Sn, Snk = S[(c + 1) % 2], f"g_S{(c + 1) % 2}"
                    q0, q0k = pq[0], "g_pq0"
                    q1, q1k = pq[1], "g_pq1"
                    P.op("pe", lambda e, B=B, Sc=Sc: e.matmul(q0[:, 0:128], B["wT"][:], Sc[:], start=True, stop=True), reads=[K_["wT"], Sk], writes=[q0k])
                    P.op("dve", lambda e, B=B: e.tensor_tensor(out=B["vnew"][:], in0=B["u"][:], in1=q0[:, 0:128], op=ALU.subtract), reads=[q0k, K_["u"]], writes=[K_["vnew"]])
                    P.op("pe", lambda e, qc=qc, Sc=Sc: e.matmul(q1[:, 0:128], qc, Sc[:], start=True, stop=True), reads=rq + [Sk], writes=[q1k])
                    P.op("act", lambda e, cB=cB, B=B: e.activation(out=B["o1"][:], in_=q1[:, 0:128], func=AF.Identity, scale=cB["eg"]), reads=[q1k, "g_eg"], writes=[K_["o1"]])
                    P.op("pe", lambda e, B=B: e.matmul(q0[:, 0:128], B["qkT"][:], B["vnew"][:], start=True, stop=True), reads=[K_["qkT"], K_["vnew"]], writes=[q0k])
                    P.op("dve", lambda e, B=B, Od=Od, c=c: e.tensor_tensor(out=Od[:, c, :], in0=B["o1"][:], in1=q0[:, 0:128], op=ALU.add), reads=[q0k, K_["o1"]], writes=[(Odk, c)])
                    P.op("pe", lambda e, B=B: e.matmul(q1[:, 0:128], B["kdec"][:], B["vnew"][:], start=True, stop=True), reads=[K_["kdec"], K_["vnew"]], writes=[q1k])
                    P.op("dve", lambda e, cB=cB, Sc=Sc, Sn=Sn: e.scalar_tensor_tensor(out=Sn[:], in0=Sc[:], scalar=cB["gtot"], in1=q1[:, 0:128], op0=ALU.mult, op1=ALU.add), reads=[q1k, Sk, "g_gtot"], writes=[Snk])
            P.dma("sp", X[:], zs[R_ZG + hl * 128:R_ZG + (hl + 1) * 128, :], writes=["g_X"])
            P.op("act", lambda e: e.activation(out=QT[:], in_=X[:], func=AF.Sigmoid), reads=["g_X"], writes=["g_QT"])
            P.op("pool", lambda e: e.tensor_tensor(out=X[:], in0=X[:], in1=QT[:], op=ALU.mult), reads=["g_X", "g_QT"], writes=["g_X"])
            for c in range(NT):
                cb = (1 - c) if c < 2 else (35 - c)
                p_, pk = npp()
                P.op("pe", lambda e, p_=p_, cb=cb: e.matmul(p_[:, 0:128], Jm, Ob[:, cb, :], start=True, stop=True), reads=[("g_Ob", cb), "g_const"], writes=[pk])
                P.op("dve", lambda e, p_=p_, c=c: e.tensor_tensor(out=osum[:], in0=Of[:, c, :], in1=p_[:, 0:128], op=ALU.add), reads=[pk, ("g_Of", c)], writes=["g_osum"])
                P.op("act", lambda e: e.activation(out=on[:], in_=osum[:], func=AF.Square, accum_out=ss[:, 0:1]), reads=["g_osum"], writes=["g_on", "g_ss"])
                P.op("act", lambda e: e.activation(out=ss[:, 1:2], in_=ss[:, 0:1], func=AF.Sqrt, scale=1.0 / 128, bias=1e-6), reads=["g_ss"], writes=["g_ss"])
                P.op("dve", lambda e: e.reciprocal(out=ss[:, 1:2], in_=ss[:, 1:2]), reads=["g_ss"], writes=["g_ss"])
                P.op("dve", lambda e: e.tensor_scalar(out=on[:], in0=osum[:], scalar1=ss[:, 1:2], scalar2=None, op0=ALU.mult), reads=["g_osum", "g_ss"], writes=["g_on"])
                p_, pk = npp()
                P.op("pe", lambda e, p_=p_: e.transpose(p_[:, 0:128], on[:], ident), reads=["g_on", "g_const"], writes=[pk])
                P.op("dve", lambda e, p_=p_, c=c: e.scalar_tensor_tensor(out=KT[:, c * 128:(c + 1) * 128], in0=p_[:, 0:128], scalar=gn[:, 0:1], in1=X[:, c * 128:(c + 1) * 128], op0=ALU.mult, op1=ALU.mult), reads=[pk, "g_gn", "g_X"], writes=["g_KT"])
            P.dma("sp", yT[1, hl * 128:(hl + 1) * 128, :], KT[:], reads=["g_KT"], is_output=True)


NE = 32
HALVES = [[0, 1], [2], [3], [4]]


def build_k4(nc):
    hT = nc.dram_tensor("hT", [128, NCH, TL], F32, kind="ExternalInput").ap()
    al_d = nc.dram_tensor("al", [128, NCH, 4], F32, kind="ExternalInput").ap()
    oT = nc.dram_tensor("oT", [128, NCH, TL], F32, kind="ExternalOutput").ap()
    with ExitStack() as st:
        P = Prog(nc, st)
        al = P.sb([128, NCH, 4], F32, "al")
        ones = P.sb([128, 128], BF16, "ones")
        P.dma("sp", al[:], al_d, writes=["al"])
        P.op("pool", lambda e: e.memset(ones[:], 1.0), writes=["ones"])
        emit_normmod(P, nc, lambda t0, n: hT[:, :, t0:t0 + n], al, ones, None, tag="n4", out_dram=oT)
        P.finish()
    return nc


def build_k3(nc):
    yT4 = nc.dram_tensor("yT4", [4, 512, TL], F32, kind="ExternalInput").ap()
    mgT = nc.dram_tensor("mgT", [4 * D, TL], F32, kind="ExternalInput").ap()
    hT = nc.dram_tensor("hT", [128, NCH, TL], F32, kind="ExternalInput").ap()
    vec3 = nc.dram_tensor("vec3", [128, NCH, 9], F32, kind="ExternalInput").ap()
    wbr = nc.dram_tensor("wbr", [4, 512, D], F32, kind="ExternalInput").ap()
    wout = nc.dram_tensor("wout", [D, D], F32, kind="ExternalInput").ap()
    rw = nc.dram_tensor("rw", [D, NE], F32, kind="ExternalInput").ap()
    rb = nc.dram_tensor("rb", [128, NE], F32, kind="ExternalInput").ap()
    w1 = nc.dram_tensor("w1", [NE, D, 1024], F32, kind="ExternalInput").ap()
    b1T = nc.dram_tensor("b1T", [128, NE, 8], F32, kind="ExternalInput").ap()
    w2 = nc.dram_tensor("w2", [NE, 512, D], F32, kind="ExternalInput").ap()
    b2 = nc.dram_tensor("b2", [NE, D], F32, kind="ExternalInput").ap()
    sel_d = nc.dram_tensor("sel", [NE, NE, 128], F32, kind="ExternalInput").ap()
    ident_d = nc.dram_tensor("ident", [128, 128], F32, kind="ExternalInput").ap()
    h1T = nc.dram_tensor("h1T", [128, NCH, TL], F32, kind="ExternalOutput").ap()
    h2T = nc.dram_tensor("h2T", [128, NCH, TL], F32, kind="ExternalOutput").ap()
    with ExitStack() as st:
        P = Prog(nc, st)
        vt = P.sb([128, NCH, 9], F32, "vt")
        al = P.sb([128, NCH, 4], F32, "al")
        ones = P.sb([128, 128], BF16, "ones")
        P.dma("sp", vt[:], vec3, writes=["vt"])
        P.op("pool", lambda e: e.memset(ones[:], 1.0), writes=["ones"])
        P.op("dve", lambda e: e.scalar_tensor_tensor(out=al[:, :, 0], in0=vt[:, :, 2], scalar=1.0, in1=vt[:, :, 8], op0=ALU.add, op1=ALU.mult), reads=["vt"], writes=["al"])
        P.op("dve", lambda e: e.tensor_copy(out=al[:, :, 1], in_=vt[:, :, 1]), reads=["vt"], writes=["al"])
        P.op("dve", lambda e: e.scalar_tensor_tensor(out=al[:, :, 2], in0=vt[:, :, 6], scalar=1.0, in1=vt[:, :, 8], op0=ALU.add, op1=ALU.mult), reads=["vt"], writes=["al"])
        P.op("dve", lambda e: e.tensor_copy(out=al[:, :, 3], in_=vt[:, :, 5]), reads=["vt"], writes=["al"])
        with Sub(P):
            mrg = P.sb([128, NCH, TL], BF16, "mrg")
            with Sub(P):
                ybf = P.sb([128, 16, TL], BF16, "ybf")
                yst = [P.sb([128, 16, 128], F32, f"yst{i}") for i in range(2)]
                wst = [P.sb([128, 16, 128], F32, f"wst{i}") for i in range(2)]
                wbf = [P.sb([128, 16, 128], BF16, f"wbf{i}") for i in range(2)]
                gl = [P.sb([128, 512], F32, f"gl{i}") for i in range(3)]
                macc = P.sb([128, 512], F32, "macc")
                tmp = P.sb([128, 512], F32, "tmpm")
                pm = [P.ps([128, 512], F32, f"pm{i}") for i in range(4)]
                yv = yT4.rearrange("i (k p) t -> p (i k) t", p=128)
                for tt in range(TL // 128):
                    i2 = tt % 2
                    P.dma("sp" if i2 == 0 else "pool", yst[i2][:], yv[:, :, tt * 128:(tt + 1) * 128], writes=[f"yst{i2}"])
                    if i2 == 0:
                        P.op("act", lambda e, tt=tt, i2=i2: e.copy(out=ybf[:, :, tt * 128:(tt + 1) * 128], in_=yst[i2][:]), reads=[f"yst{i2}"], writes=[("ybf", tt)])
                    else:
                        P.op("dve", lambda e, tt=tt, i2=i2: e.tensor_copy(out=ybf[:, :, tt * 128:(tt + 1) * 128], in_=yst[i2][:]), reads=[f"yst{i2}"], writes=[("ybf", tt)])
                ybk = [("ybf", tt) for tt in range(TL // 128)]
                wbv = wbr.rearrange("i (k p) c -> p (i k) c", p=128)
                n_ = 0
                for j in range(NCH):
                    j2 = j % 2
                    P.dma("sp", wst[j2][:], wbv[:, :, j * 128:(j + 1) * 128], writes=[f"wst{j2}"])
                    P.op("pool", lambda e, j2=j2: e.tensor_copy(out=wbf[j2][:], in_=wst[j2][:]), reads=[f"wst{j2}"], writes=[f"wbf{j2}"])
                    for bi, (t0, n) in enumerate(BLOCKS):
                        for i in range(4):
                            p_ = pm[n_ % 4]
                            pk = f"pm{n_ % 4}"
                            g_ = gl[n_ % 3]
                            gk = f"gl{n_ % 3}"
                            n_ += 1
                            P.dma("pool" if i % 2 else "sp", g_[:, 0:n], mgT[i * D + j * 128:i * D + (j + 1) * 128, t0:t0 + n], writes=[gk])
                            P.op("act", lambda e, g_=g_, n=n: e.activation(out=g_[:, 0:n], in_=g_[:, 0:n], func=AF.Sigmoid), reads=[gk], writes=[gk])
                            for k in range(4):
                                P.op("pe", lambda e, p_=p_, j2=j2, i=i, k=k, t0=t0, n=n: e.matmul(p_[:, 0:n], wbf[j2][:, i * 4 + k, :], ybf[:, i * 4 + k, t0:t0 + n], start=(k == 0), stop=(k == 3)),
                                     reads=[f"wbf{j2}"] + ybk, writes=[pk])
                            if i == 0:
                                P.op("dve", lambda e, p_=p_, g_=g_, n=n: e.tensor_tensor(out=macc[:, 0:n], in0=p_[:, 0:n], in1=g_[:, 0:n], op=ALU.mult), reads=[pk, gk], writes=["macc"])
                            else:
                                P.op("dve", lambda e, p_=p_, g_=g_, n=n: e.tensor_tensor(out=tmp[:, 0:n], in0=p_[:, 0:n], in1=g_[:, 0:n], op=ALU.mult), reads=[pk, gk], writes=["tmpm"])
                                if i < 3:
                                    P.op("pool", lambda e, n=n: e.tensor_tensor(out=macc[:, 0:n], in0=macc[:, 0:n], in1=tmp[:, 0:n], op=ALU.add), reads=["macc", "tmpm"], writes=["macc"])
                                else:
                                    P.op("pool", lambda e, n=n, j=j, t0=t0: e.tensor_tensor(out=mrg[:, j, t0:t0 + n], in0=macc[:, 0:n], in1=tmp[:, 0:n], op=ALU.add), reads=["macc", "tmpm"], writes=[("mrg", j, bi)])
            with Sub(P):
                owst_l = [P.sb([128, 16, 128], F32, f"owst{i}") for i in range(2)]
                owbf_l = [P.sb([128, 16, 128], BF16, f"owbf{i}") for i in range(2)]
                ohb_l = [P.sb([128, 512], F32, f"ohb{i}") for i in range(3)]
                opo_l = [P.ps([128, 512], F32, f"po{i}") for i in range(4)]
                wov = wout.rearrange("(k p) c -> p k c", p=128)
                n_ = 0
                for j in range(NCH):
                    j2 = j % 2
                    P.dma("sp", owst_l[j2][:], wov[:, :, j * 128:(j + 1) * 128], writes=[f"owst{j2}"])
                    P.op("pool", lambda e, j2=j2: e.tensor_copy(out=owbf_l[j2][:], in_=owst_l[j2][:]), reads=[f"owst{j2}"], writes=[f"owbf{j2}"])
                    for bi, (t0, n) in enumerate(BLOCKS):
                        p_ = opo_l[n_ % 4]
                        pk = f"po{n_ % 4}"
                        h_ = ohb_l[n_ % 3]
                        hk = f"ohb{n_ % 3}"
                        n_ += 1
                        P.dma("pool", h_[:, 0:n], hT[:, j, t0:t0 + n], writes=[hk])
                        for k in range(NCH):
                            P.op("pe", lambda e, p_=p_, j2=j2, k=k, t0=t0, n=n: e.matmul(p_[:, 0:n], owbf_l[j2][:, k, :], mrg[:, k, t0:t0 + n], start=(k == 0), stop=(k == NCH - 1)),
                                 reads=[f"owbf{j2}"] + [("mrg", k, bi)], writes=[pk])
                        gi = 4 if bi == 0 else 0
                        P.op("dve", lambda e, p_=p_, h_=h_, n=n, j=j, gi=gi: e.scalar_tensor_tensor(out=h_[:, 0:n], in0=p_[:, 0:n], scalar=vt[:, j, gi:gi + 1], in1=h_[:, 0:n], op0=ALU.mult, op1=ALU.add), reads=[pk, hk, "vt"], writes=[hk])
                        P.dma("sp", h1T[:, j, t0:t0 + n], h_[:, 0:n], reads=[hk], is_output=True)
        def moe_group(hf, blks):
            T0 = BLOCKS[blks[0]][0]
            NTK = sum(BLOCKS[b][1] for b in blks)
            with Sub(P):
                hn = P.sb([128, NCH, TL], BF16, f"hn2_{hf}") if False else None
                hnh = P.sb([128, NCH, NTK], BF16, f"hnh{hf}")

                class HV:
                    def __init__(self, t, o):
                        self.t, self.o = t, o
                    def __getitem__(self, idx):
                        p, k, ts = idx
                        return self.t[p, k, slice(ts.start - self.o, ts.stop - self.o)]
                emit_normmod(P, nc, lambda t0, n: h1T[:, :, t0:t0 + n], al, ones, HV(hnh, T0), tag=f"n2{hf}", blocks=[(b, BLOCKS[b]) for b in blks])
                acc = P.sb([128, NCH, NTK], F32, "acc")
                GT = P.sb([NE, NTK], F32, "GT")
                sel = P.sb([NE, NE, 128], F32, "sel")
                ident = P.sb([128, 128], F32, "ident")
                rwst = P.sb([128, NCH, NE], F32, "rwst")
                rwbf = P.sb([128, NCH, NE], BF16, "rwbf")
                rbs = P.sb([128, NE], F32, "rbs")
                b1s = P.sb([128, NE, 8], F32, "b1s")
                b2s = P.sb([NE, D], F32, "b2s")
                lg = P.sb([128, NE], F32, "lg")
                ex = P.sb([128, NE], F32, "ex")
                mk = P.sb([128, NE], F32, "mk")
                m8 = P.sb([128, 8], F32, "m8")
                sm = P.sb([128, 4], F32, "sm")
                w1st = [P.sb([128, 1024], F32, f"w1st{i}") for i in range(3)]
                w1bf = P.sb([128, NCH, 1024], BF16, "w1bf")
                w2bf = P.sb([128, 4, D], BF16, "w2bf")
                gb = P.sb([128, 512], F32, "gb")
                tg = [P.sb([128, 512], F32, f"tg{i}") for i in range(2)]
                tsg = [P.sb([128, 512], F32, f"tsg{i}") for i in range(2)]
                tl_ = [P.sb([128, 512], F32, f"tl{i}") for i in range(2)]
                gact = [P.sb([128, 4, 512], BF16, f"gact{i}") for i in range(2)]
                hb = [P.sb([128, 512], F32, f"fhb{i}") for i in range(1)]
                pg = [P.ps([128, 512], F32, f"pg{i}") for i in range(2)]
                pl = [P.ps([128, 512], F32, f"pl{i}") for i in range(2)]
                pgb = P.ps([128, 512], F32, "pgb")
                po = [P.ps([128, 512], F32, f"pout{i}") for i in range(2)]
                pr = P.ps([128, 512], F32, "pr")
                P.dma("sp", sel[:], sel_d, writes=["sel"])
                P.dma("sp", ident[:], ident_d, writes=["ident"])
                P.dma("sp", rwst[:], rw.rearrange("(k p) e -> p k e", p=128), writes=["rwst"])
                P.dma("sp", rbs[:], rb, writes=["rbs"])
                P.dma("sp", b1s[:], b1T, writes=["b1s"])
                P.dma("sp", b2s[:], b2, writes=["b2s"])
                P.op("dve", lambda e: e.tensor_copy(out=rwbf[:], in_=rwst[:]), reads=["rwst"], writes=["rwbf"])
                hkeys = [("hn", b) for b in blks]
                for tt in range(NTK // 128):
                    c0 = tt * 128
                    for k in range(NCH):
                        P.op("pe", lambda e, k=k, c0=c0: e.matmul(pr[:, 0:NE], hnh[:, k, c0:c0 + 128], rwbf[:, k, :], start=(k == 0), stop=(k == NCH - 1)), reads=hkeys + ["rwbf"], writes=["pr"])
                    P.op("dve", lambda e: e.tensor_tensor(out=lg[:], in0=pr[:, 0:NE], in1=rbs[:], op=ALU.add), reads=["pr", "rbs"], writes=["lg"])
                    P.op("dve", lambda e: e.max(out=m8[:], in_=lg[:]), reads=["lg"], writes=["m8"])
                    P.op("dve", lambda e: e.tensor_scalar(out=sm[:, 0:1], in0=m8[:, 0:1], scalar1=-1.0, scalar2=None, op0=ALU.mult), reads=["m8"], writes=["sm"])
                    P.op("act", lambda e: e.activation(out=ex[:], in_=lg[:], func=AF.Exp, bias=sm[:, 0:1]), reads=["lg", "sm"], writes=["ex"])
                    P.op("dve", lambda e: e.tensor_scalar(out=mk[:], in0=lg[:], scalar1=m8[:, 3:4], scalar2=None, op0=ALU.is_ge), reads=["lg", "m8"], writes=["mk"])
                    P.op("dve", lambda e: e.tensor_tensor(out=ex[:], in0=ex[:], in1=mk[:], op=ALU.mult), reads=["ex", "mk"], writes=["ex"])
                    P.op("dve", lambda e: e.reduce_sum(out=sm[:, 1:2], in_=ex[:], axis=AX.X), reads=["ex"], writes=["sm"])
                    P.op("dve", lambda e: e.reciprocal(out=sm[:, 2:3], in_=sm[:, 1:2]), reads=["sm"], writes=["sm"])
                    P.op("dve", lambda e: e.tensor_scalar(out=ex[:], in0=ex[:], scalar1=sm[:, 2:3], scalar2=None, op0=ALU.mult), reads=["ex", "sm"], writes=["ex"])
                    P.op("pe", lambda e: e.transpose(pr[0:NE, 128:256], ex[:], ident[:]), reads=["ex", "ident"], writes=["pr"])
                    P.op("act", lambda e, c0=c0: e.copy(out=GT[:, c0:c0 + 128], in_=pr[0:NE, 128:256]), reads=["pr"], writes=["GT"])
                lblocks = []
                c0 = 0
                for b in blks:
                    lblocks.append((c0, BLOCKS[b][1], b))
                    c0 += BLOCKS[b][1]
                n_ = 0
                for (c0, n, b) in lblocks:
                    for j in range(NCH):
                        p_ = po[n_ % 2]
                        pk = f"pout{n_ % 2}"
                        n_ += 1
                        P.op("pe", lambda e, p_=p_, j=j, c0=c0, n=n: e.matmul(p_[:, 0:n], b2s[:, j * 128:(j + 1) * 128], GT[:, c0:c0 + n], start=True, stop=True), reads=["b2s", "GT"], writes=[pk])
                        P.op("act", lambda e, p_=p_, j=j, c0=c0, n=n: e.copy(out=acc[:, j, c0:c0 + n], in_=p_[:, 0:n]), reads=[pk], writes=[("acc", j, b)])
                cnt = 0
                for ex_i in range(NE):
                    w1v = w1[ex_i].rearrange("(k p) f -> p k f", p=128)
                    for q4 in range(16):
                        s_ = w1st[q4 % 3]
                        sk = f"w1st{q4 % 3}"
                        P.dma("sp" if q4 % 2 == 0 else "pool", s_[:], w1v[:, q4, :], writes=[sk])
                        eng = ("pool", "dve", "pool", "act")[q4 % 4]
                        if eng == "act":
                            P.op("act", lambda e, s_=s_, q4=q4: e.copy(out=w1bf[:, q4, :], in_=s_[:]), reads=[sk], writes=[("w1bf", q4)])
                        else:
                            P.op(eng, lambda e, s_=s_, q4=q4: e.tensor_copy(out=w1bf[:, q4, :], in_=s_[:]), reads=[sk], writes=[("w1bf", q4)])
                    for f8 in range(8):
                        f, hh = f8 // 2, f8 % 2
                        s_ = w1st[(f8 + 1) % 3]
                        sk = f"w1st{(f8 + 1) % 3}"
                        P.dma("sp" if f8 % 2 == 0 else "pool", s_[:], w2[ex_i, f * 128:(f + 1) * 128, hh * 1024:(hh + 1) * 1024], writes=[sk])
                        P.op("pool" if f8 % 2 else "dve", lambda e, s_=s_, f=f, hh=hh: e.tensor_copy(out=w2bf[:, f, hh * 1024:(hh + 1) * 1024], in_=s_[:]), reads=[sk], writes=[("w2bf", f8)])
                    w1k = [("w1bf", q) for q in range(16)]
                    w2k = [("w2bf", q) for q in range(8)]
                    for (c0, n, b) in lblocks:
                        ga = gact[cnt % 2]
                        gak = f"gact{cnt % 2}"
                        cnt += 1
                        P.op("pe", lambda e, ex_i=ex_i, c0=c0, n=n: e.matmul(pgb[:, 0:n], sel[:, ex_i, :], GT[:, c0:c0 + n], start=True, stop=True), reads=["sel", "GT"], writes=["pgb"])
                        P.op("act", lambda e, n=n: e.copy(out=gb[:, 0:n], in_=pgb[:, 0:n]), reads=["pgb"], writes=["gb"])
                        for f in range(4):
                            i2 = f % 2
                            for k in range(NCH):
                                P.op("pe", lambda e, i2=i2, k=k, f=f, c0=c0, n=n: e.matmul(pg[i2][:, 0:n], w1bf[:, k, f * 128:(f + 1) * 128], hnh[:, k, c0:c0 + n], start=(k == 0), stop=(k == NCH - 1)), reads=w1k + hkeys, writes=[f"pg{i2}"])
                            for k in range(NCH):
                                P.op("pe", lambda e, i2=i2, k=k, f=f, c0=c0, n=n: e.matmul(pl[i2][:, 0:n], w1bf[:, k, 512 + f * 128:512 + (f + 1) * 128], hnh[:, k, c0:c0 + n], start=(k == 0), stop=(k == NCH - 1)), reads=w1k + hkeys, writes=[f"pl{i2}"])
                            P.op("dve", lambda e, i2=i2, f=f, n=n, ex_i=ex_i: e.tensor_scalar(out=tg[i2][:, 0:n], in0=pg[i2][:, 0:n], scalar1=b1s[:, ex_i, f:f + 1], scalar2=7.0, op0=ALU.add, op1=ALU.min), reads=[f"pg{i2}", "b1s"], writes=[f"tg{i2}"])
                            P.op("act", lambda e, i2=i2, n=n: e.activation(out=tsg[i2][:, 0:n], in_=tg[i2][:, 0:n], func=AF.Sigmoid, scale=1.702), reads=[f"tg{i2}"], writes=[f"tsg{i2}"])
                            P.op("dve", lambda e, i2=i2, f=f, n=n, ex_i=ex_i: e.tensor_scalar(out=tl_[i2][:, 0:n], in0=pl[i2][:, 0:n], scalar1=b1s[:, ex_i, 4 + f:5 + f], scalar2=7.0, op0=ALU.add, op1=ALU.min), reads=[f"pl{i2}", "b1s"], writes=[f"tl{i2}"])
                            P.op("pool", lambda e, i2=i2, n=n: e.tensor_scalar(out=tl_[i2][:, 0:n], in0=tl_[i2][:, 0:n], scalar1=-7.0, scalar2=1.0, op0=ALU.max, op1=ALU.add), reads=[f"tl{i2}"], writes=[f"tl{i2}"])
                            P.op("pool", lambda e, i2=i2, n=n: e.tensor_tensor(out=tg[i2][:, 0:n], in0=tg[i2][:, 0:n], in1=tsg[i2][:, 0:n], op=ALU.mult), reads=[f"tg{i2}", f"tsg{i2}"], writes=[f"tg{i2}"])
                            P.op("pool", lambda e, i2=i2, n=n: e.tensor_tensor(out=tl_[i2][:, 0:n], in0=tl_[i2][:, 0:n], in1=gb[:, 0:n], op=ALU.mult), reads=[f"tl{i2}", "gb"], writes=[f"tl{i2}"])
                            P.op("dve", lambda e, i2=i2, n=n, f=f, ga=ga: e.tensor_tensor(out=ga[:, f, 0:n], in0=tg[i2][:, 0:n], in1=tl_[i2][:, 0:n], op=ALU.mult), reads=[f"tg{i2}", f"tl{i2}"], writes=[gak])
                        for j in range(NCH):
                            p_ = po[j % 2]
                            pk = f"pout{j % 2}"
                            for f in range(4):
                                P.op("pe", lambda e, p_=p_, f=f, j=j, n=n, ga=ga: e.matmul(p_[:, 0:n], w2bf[:, f, j * 128:(j + 1) * 128], ga[:, f, 0:n], start=(f == 0), stop=(f == 3)), reads=w2k + [gak], writes=[pk])
                            P.op("dve", lambda e, p_=p_, j=j, c0=c0, n=n: e.tensor_tensor(out=acc[:, j, c0:c0 + n], in0=p_[:, 0:n], in1=acc[:, j, c0:c0 + n], op=ALU.add), reads=[pk, ("acc", j, b)], writes=[("acc", j, b)])
                n_ = 0
                for (c0, n, b) in lblocks:
                    t0 = BLOCKS[b][0]
                    gi = 7 if b == 0 else 3
                    for j in range(NCH):
                        h_ = hb[0]
                        hk = "fhb0"
                        n_ += 1
                        P.dma("pool", h_[:, 0:n], h1T[:, j, t0:t0 + n], writes=[hk])
                        P.op("dve", lambda e, h_=h_, j=j, c0=c0, n=n, gi=gi: e.scalar_tensor_tensor(out=h_[:, 0:n], in0=acc[:, j, c0:c0 + n], scalar=vt[:, j, gi:gi + 1], in1=h_[:, 0:n], op0=ALU.mult, op1=ALU.add), reads=[("acc", j, b), hk, "vt"], writes=[hk])
                        P.dma("sp", h2T[:, j, t0:t0 + n], h_[:, 0:n], reads=[hk], is_output=True)

        for hf_, blks_ in enumerate(HALVES):
            moe_group(hf_, blks_)
        P.finish()
    return nc


_CONST = {}

def consts():
    if _CONST:
        return _CONST
    Tn = 4096
    t = np.arange(Tn, dtype=np.int64)
    prod = (t[:, None] * t[None, :]) % Tn
    ang = prod.astype(np.float64) * (2 * np.pi / Tn)
    _CONST['dftc'] = np.cos(ang).astype(np.float32).astype(ml_dtypes.bfloat16)
    _CONST['dfts'] = np.sin(ang).astype(np.float32).astype(ml_dtypes.bfloat16)
    c = np.arange(128, dtype=np.int64)
    angc = ((c[:, None] * c[None, :]) % 128).astype(np.float64) * (2 * np.pi / 128)
    _CONST['chan_cs'] = np.concatenate([np.cos(angc), -np.sin(angc)], 1).astype(np.float32)
    inv = np.power(10000.0, -np.arange(16, dtype=np.float32) / 16).astype(np.float32)
    pos = np.arange(4096)
    row = (pos // 64).astype(np.float32); col = (pos % 64).astype(np.float32)
    ar = (row[None, :] * inv[:, None]).astype(np.float32); ac = (col[None, :] * inv[:, None]).astype(np.float32)
    COS = np.concatenate([np.cos(ar), np.cos(ar), np.cos(ac), np.cos(ac)], 0).astype(np.float32)
    SINS = np.concatenate([-np.sin(ar), np.sin(ar), -np.sin(ac), np.sin(ac)], 0).astype(np.float32)
    _CONST['rope_cos'] = COS; _CONST['rope_sin'] = SINS
    return _CONST

SW = np.concatenate([np.arange(16, 32), np.arange(0, 16), np.arange(48, 64), np.arange(32, 48)])

def zsel_rows(p):
    r = []
    blk = lambda base, i: list(range(base + i * 128, base + (i + 1) * 128))
    for base in (0, 512):
        for i in (2 * p, 2 * p + 1):
            r += blk(base, i)
    for base in (1024, 1536, 2048, 2560):
        for i in (2 * p, 2 * p + 1):
            r += blk(base, i)
    r += list(range(3072, 3088))
    for i in (2 * p, 2 * p + 1):
        r += blk(3088, i)
    r += list(range(3600, 4432))
    r += [4368 + int(i) for i in SW]
    assert len(r) == 2704
    return np.array(r)

def pp(v):
    return np.ascontiguousarray(v.reshape(-1, 128).T)

def k2_weights(W, l, p):
    o = {}
    hs = (2 * p, 2 * p + 1)
    cw = W['lru_conv_w'][l]; cb = W['lru_conv_b'][l]
    o['lru_cw'] = np.stack([np.concatenate([cw[:, g * 128:(g + 1) * 128].T, cb[g * 128:(g + 1) * 128, None]], 1) for g in hs], 1).astype(np.float32)
    wa = W['lru_w_a'][l]; wx = W['lru_w_x'][l]
    o['lru_wg'] = np.ascontiguousarray(np.stack([np.stack([np.stack([wa[d, g], wx[d, g]], 1) for d in range(2)], 1) for g in hs], 1)).astype(np.float32)
    ba = W['lru_b_a'][l]; bx = W['lru_b_x'][l]; lam = W['lru_lambda'][l]
    o['lru_vec'] = np.stack([np.stack([np.stack([ba[d, g * 128:(g + 1) * 128], bx[d, g * 128:(g + 1) * 128], lam[d, g * 128:(g + 1) * 128]], -1) for d in range(2)], 1) for g in hs], 1).astype(np.float32)
    return {k: np.ascontiguousarray(v) for k, v in o.items()}

def k2_weights_mla(W, l, p):
    o = {}
    hs = (2 * p, 2 * p + 1)
    o['mla_g'] = np.concatenate([pp(W['mla_q_norm_g'][l]), pp(W['mla_kv_norm_g'][l])], 1).astype(np.float32)
    wuq = W['mla_w_uq'][l]; wukv = W['mla_w_ukv'][l]
    o['wuq'] = np.concatenate([wuq[:, h * 192:(h + 1) * 192] for h in hs], 1)
    o['wuq_sw'] = np.concatenate([wuq[:, h * 192 + 128:(h + 1) * 192][:, SW] for h in hs], 1)
    o['wukv'] = np.concatenate([wukv[:, h * 256:(h + 1) * 256] for h in hs], 1)
    return {k: np.ascontiguousarray(v) for k, v in o.items()}

def gdn_consts():
    i = np.arange(128)
    ident = np.eye(128, dtype=np.float32)
    J = ident[::-1].copy()
    tri = (i[:, None] <= i[None, :]).astype(np.float32)
    strict = (i[:, None] > i[None, :]).astype(np.float32)
    ones = np.ones((128, 128), np.float32)
    return np.ascontiguousarray(np.stack([ident, J, tri, strict, tri, ones], 1))

def rev_index():
    return np.concatenate([np.arange(255, -1, -1), np.arange(4351, 255, -1)])

def k2_weights_gdn(W, l, p, zfullT):
    o = {}
    hs = (2 * p, 2 * p + 1)
    cw = W['gdn_conv_w'][l]; cb = W['gdn_conv_b'][l]
    tiles = []
    for base in (0, 512, 1024):
        for h in hs:
            sl = slice(base + h * 128, base + (h + 1) * 128)
            tiles.append(np.concatenate([cw[:, sl].T, cb[sl, None]], 1))
    o['gdn_cw'] = np.stack(tiles, 1).astype(np.float32)
    al = W['gdn_a_log'][l]; dtb = W['gdn_dt_bias'][l]
    sc = np.zeros((128, 4, 2), np.float32)
    for d in range(2):
        for hl, h in enumerate(hs):
            sc[:, d * 2 + hl, 0] = al[d, h]; sc[:, d * 2 + hl, 1] = dtb[d, h]
    o['gdn_sc'] = sc
    o['gdn_gn'] = W['gdn_norm_g'][l].reshape(128, 1).astype(np.float32)
    ri = rev_index()
    bg = np.zeros((128, 34, 8), np.float32)
    for d in range(2):
        for hl, h in enumerate(hs):
            brow = zfullT[3072 + d * 4 + h]; arow = zfullT[3080 + d * 4 + h]
            if d == 1:
                brow = brow[ri]; arow = arow[ri]
            bg[:, :, d * 2 + hl] = brow.reshape(34, 128).T
            bg[:, :, 4 + d * 2 + hl] = arow.reshape(34, 128).T
    o['bg_tm'] = bg
    o['gconst'] = gdn_consts()
    return {k: np.ascontiguousarray(v) for k, v in o.items()}


def fm(v):
    return np.ascontiguousarray(v.reshape(16, 128).T)

def k3_consts():
    sel = np.zeros((32, 32, 128), np.float32)
    for e in range(32):
        sel[e, e, :] = 1.0
    return sel, np.eye(128, dtype=np.float32)

def k3_weights(W, l):
    o = {}
    o['wbr'] = W['w_branch'][l]; o['wout'] = W['w_out'][l]; o['rw'] = W['router_w'][l]
    o['rb'] = np.broadcast_to(W['router_b'][l][None, :], (128, 32)).copy()
    o['w1'] = W['exp_w1'][l]; o['w2'] = W['exp_w2'][l]; o['b2'] = W['exp_b2'][l]
    o['b1T'] = np.ascontiguousarray(W['exp_b1'][l].reshape(32, 8, 128).transpose(2, 0, 1))
    sel, ident = k3_consts()
    o['sel'] = sel; o['ident'] = ident
    return {k: np.ascontiguousarray(v, dtype=np.float32) for k, v in o.items()}


TLc = 2176


def _fm(v):
    return np.ascontiguousarray(np.asarray(v, dtype=np.float32).reshape(16, 128).T)


def _to_fm(h_loc):
    return np.ascontiguousarray(h_loc.T.reshape(16, 128, TLc).transpose(1, 0, 2))


def _from_fm(a):
    return a.transpose(1, 0, 2).reshape(2048, TLc).T


_PROGS = {}


def _prog(name, builder):
    if name not in _PROGS:
        nc = bass.Bass("TRN2", target_bir_lowering=False)
        builder(nc)
        _PROGS[name] = nc
    return _PROGS[name]


def build_k2(nc):
    Cc = consts()
    zs_d = nc.dram_tensor("zs", [R_END, T], F32, kind="ExternalInput").ap()
    yT = nc.dram_tensor("yT", [4, 256, T], F32, kind="ExternalOutput").ap()

    def inp(name, shape, dt=F32):
        return nc.dram_tensor(name, list(shape), dt, kind="ExternalInput").ap()
    with ExitStack() as st:
        P = Prog(nc, st)
        emit_fft(P, zs_d, yT, inp("dftc", [4096, 4096], BF16), inp("dfts", [4096, 4096], BF16), inp("chan_cs", [128, 256]))
        emit_lru(P, zs_d, yT, inp("lru_cw", [128, 2, 5]), inp("lru_wg", [128, 2, 2, 2, 128]), inp("lru_vec", [128, 2, 2, 3]))
        emit_mla(P, zs_d, yT, inp("mla_g", [128, 6]), inp("wuq", [512, 384]), inp("wuq_sw", [512, 128]), inp("wukv", [256, 512]), inp("rope_cos", [64, 4096]), inp("rope_sin", [64, 4096]))
        emit_gdn(P, zs_d, yT, inp("gdn_cw", [128, 6, 5]), inp("gdn_sc", [128, 4, 2]), inp("gdn_gn", [128, 1]), inp("bg_tm", [128, 34, 8]), inp("gconst", [128, 6, 128]))
        P.finish()
    return nc


def kernel(**inputs):
    W = {k: np.asarray(v) for k, v in inputs.items()}
    NCORE = 8
    cores = list(range(NCORE))
    Cc = consts()
    wada = np.ascontiguousarray(W['w_ada'].transpose(1, 0, 2).reshape(2048, 4 * 12288))
    bada = W['b_ada'].reshape(4 * 12288)
    cTm = np.zeros((2048, 8), np.float32)
    cTm[:, 0:4] = W['c'].T
    cTm[:, 4] = W['c_ctx']
    cT = np.ascontiguousarray(cTm.reshape(16, 128, 8).transpose(1, 0, 2))
    ins = []
    for c in cores:
        ins.append({"w": np.ascontiguousarray(wada[:, c * 6144:(c + 1) * 6144]),
                    "bias": np.ascontiguousarray(bada[c * 6144:(c + 1) * 6144].reshape(48, 128).T), "cT": cT})
    res = run_bass_kernel_spmd(_prog("k0", build_k0), ins, core_ids=cores)
    mod = np.concatenate([r["modT"].transpose(1, 0, 2).reshape(6144, 8) for r in res.results], 0).reshape(4, 6, 2048, 8)
    hT = []
    for c in cores:
        b, s = c // 2, c % 2
        h_loc = np.concatenate([W['ctx'][b, 128 * s:128 * s + 128], W['x'][b, 2048 * s:2048 * s + 2048]], 0)
        hT.append(_to_fm(h_loc))
    sel, ident = k3_consts()
    for l in range(4):
        ins = []
        for c in cores:
            b = c // 2
            m = mod[l]
            vec = np.stack([_fm(m[0, :, b]), _fm(m[1, :, b]), _fm(m[0, :, 4]), _fm(m[1, :, 4]), _fm(W['norm1_g'][l])], -1)
            ins.append({"hT": hT[c], "vec": np.ascontiguousarray(vec), "w": W['w_in'][l]})
        res = run_bass_kernel_spmd(_prog("k1", build_k1), ins, core_ids=cores)
        zT = [r["zT"] for r in res.results]
        ins = []
        zb = {}
        for b in range(4):
            z0, z1 = zT[2 * b], zT[2 * b + 1]
            zb[b] = np.concatenate([z0[:, 0:128], z1[:, 0:128], z0[:, 128:], z1[:, 128:]], 1)
        for c in cores:
            b, p = c // 2, c % 2
            d_ = {"zs": np.ascontiguousarray(zb[b][zsel_rows(p)]), "dftc": Cc['dftc'], "dfts": Cc['dfts'], "chan_cs": Cc['chan_cs'],
                  "rope_cos": Cc['rope_cos'], "rope_sin": Cc['rope_sin']}
            d_.update(k2_weights(W, l, p))
            d_.update(k2_weights_mla(W, l, p))
            d_.update(k2_weights_gdn(W, l, p, zb[b]))
            ins.append(d_)
        res = run_bass_kernel_spmd(_prog("k2", build_k2), ins, core_ids=cores)
        yb = {}
        for b in range(4):
            y0, y1 = res.results[2 * b]["yT"], res.results[2 * b + 1]["yT"]
            yb[b] = np.concatenate([y0, y1], 1)
        del zb
        wk3 = k3_weights(W, l)
        ins = []
        for c in cores:
            b, s = c // 2, c % 2
            tok = np.concatenate([np.arange(128 * s, 128 * s + 128), 256 + np.arange(2048 * s, 2048 * s + 2048)])
            m = mod[l]
            vec3 = np.stack([_fm(m[2, :, b]), _fm(m[3, :, b]), _fm(m[4, :, b]), _fm(m[5, :, b]),
                             _fm(m[2, :, 4]), _fm(m[3, :, 4]), _fm(m[4, :, 4]), _fm(m[5, :, 4]), _fm(W['norm2_g'][l])], -1)
            d_ = {"yT4": np.ascontiguousarray(yb[b][:, :, tok]), "mgT": np.ascontiguousarray(zT[c][4432:]), "hT": hT[c], "vec3": np.ascontiguousarray(vec3)}
            d_.update(wk3)
            ins.append(d_)
        del zT
        res = run_bass_kernel_spmd(_prog("k3", build_k3), ins, core_ids=cores)
        hT = [r["h2T"] for r in res.results]
    al = np.zeros((128, 16, 4), np.float32)
    al[:, :, 0] = _fm(W['final_norm_g'])
    al[:, :, 2] = _fm(W['final_norm_g'])
    ins = [{"hT": hT[c], "al": al} for c in cores]
    res = run_bass_kernel_spmd(_prog("k4", build_k4), ins, core_ids=cores)
    out = np.zeros((4, 4096, 2048), np.float32)
    for c in cores:
        b, s = c // 2, c % 2
        o = _from_fm(res.results[c]["oT"])
        out[b, 2048 * s:2048 * s + 2048] = o[128:]
    return out
```
